# Optimizing a Trainium2 kernel written in Bass

```python
import math
import jax, jax.numpy as jnp
from jax import lax
import numpy as np

D_MODEL = 1024
BATCH = 4
SEQ = 8192
DEPTH = 2

HEAD_DIM = 64
ROPE_THETA = 10000.0
Q_BLOCK = 128
LN_EPS = 1e-5

A_HEADS = 4
A_KV_RANK = 128
A_IDX_HEADS = 4
A_IDX_DIM = 64
A_TOPK_MAX = 256

B_HEADS = 4
B_BLOCK = 256
B_TOPK_BLOCKS = 3

C_HEADS = 8
C_KV_HEADS = 2
C_WINDOW = 128

N_BRANCH = 3
A_W = A_HEADS * HEAD_DIM
B_W = B_HEADS * HEAD_DIM
C_W = C_HEADS * HEAD_DIM
MIX_W = A_W + B_W + C_W

IN_SIZES = (A_W, A_KV_RANK, A_IDX_HEADS * A_IDX_DIM, A_IDX_DIM, A_IDX_HEADS,
            B_W, B_W, B_W,
            C_W, C_KV_HEADS * HEAD_DIM, C_KV_HEADS * HEAD_DIM,
            N_BRANCH * D_MODEL)
N_IN = sum(IN_SIZES)

N_EXPERTS = 16
N_GROUPS = 4
EXPERTS_PER_GROUP = N_EXPERTS // N_GROUPS
TOP_K = 2
D_EXPERT = 512

DN_ALPHA = (2 * DEPTH) ** 0.25
DN_BETA = (8 * DEPTH) ** -0.25

kernel_name = "hybrid_dsa_moba_swa_groupmoe_deepnorm"


def _split_points(sizes):
    pts, acc = [], 0
    for s in sizes[:-1]:
        acc += s
        pts.append(acc)
    return pts


def layer_norm(x, g, b):
    xf = x.astype(jnp.float32)
    mu = jnp.mean(xf, -1, keepdims=True)
    var = jnp.mean(jnp.square(xf - mu), -1, keepdims=True)
    return ((xf - mu) * lax.rsqrt(var + LN_EPS) * g + b).astype(x.dtype)


def rope_tables(seq_len, dim):
    inv = 1.0 / (ROPE_THETA ** (jnp.arange(0, dim, 2, dtype=jnp.float32) / dim))
    ang = jnp.arange(seq_len, dtype=jnp.float32)[:, None] * inv[None, :]
    return jnp.cos(ang), jnp.sin(ang)


def apply_rope(t, cos, sin):
    t1, t2 = jnp.split(t, 2, axis=-1)
    c = cos[None, :, None, :].astype(t.dtype)
    s = sin[None, :, None, :].astype(t.dtype)
    return jnp.concatenate([t1 * c - t2 * s, t2 * c + t1 * s], axis=-1)


def dsa_attention(q, k, v, iq, ik, iw):
    Bn, S, HA, dh = q.shape
    topk = min(A_TOPK_MAX, S // 4)
    nqb = S // Q_BLOCK
    scale = dh ** -0.5
    key_pos = jnp.arange(S)
    gather = jax.vmap(lambda arr, idx: arr[idx])

    def block(i):
        t0 = i * Q_BLOCK
        qb = lax.dynamic_slice_in_dim(q, t0, Q_BLOCK, 1)
        iqb = lax.dynamic_slice_in_dim(iq, t0, Q_BLOCK, 1)
        iwb = lax.dynamic_slice_in_dim(iw, t0, Q_BLOCK, 1)
        qpos = t0 + jnp.arange(Q_BLOCK)
        dots = jnp.einsum('bqhd,bsd->bqhs', iqb, ik)
        score = jnp.einsum('bqh,bqhs->bqs', iwb, jax.nn.relu(dots)).astype(jnp.float32)
        causal = key_pos[None, :] <= qpos[:, None]
        score = jnp.where(causal[None], score, -jnp.inf)
        _, idx = lax.top_k(score, topk)
        valid = idx <= qpos[None, :, None]
        kg = gather(k, idx)
        vg = gather(v, idx)
        s = jnp.einsum('bqhd,bqkd->bhqk', qb, kg).astype(jnp.float32) * scale
        s = jnp.where(valid[:, None], s, -jnp.inf)
        p = jax.nn.softmax(s, axis=-1).astype(v.dtype)
        return jnp.einsum('bhqk,bqkd->bqhd', p, vg)

    out = lax.map(block, jnp.arange(nqb))
    return out.transpose(1, 0, 2, 3, 4).reshape(Bn, S, HA * dh)


def moba_attention(q, k, v):
    Bn, S, H, dh = q.shape
    nkb = -(-S // B_BLOCK)
    pad = nkb * B_BLOCK - S
    kp = jnp.pad(k, ((0, 0), (0, pad), (0, 0), (0, 0)))
    vp = jnp.pad(v, ((0, 0), (0, pad), (0, 0), (0, 0)))
    kblk = kp.reshape(Bn, nkb, B_BLOCK, H, dh)
    vblk = vp.reshape(Bn, nkb, B_BLOCK, H, dh)
    kmean = jnp.mean(kblk, axis=2)
    kbt = kblk.transpose(0, 3, 1, 2, 4)
    vbt = vblk.transpose(0, 3, 1, 2, 4)
    nsel = min(B_TOPK_BLOCKS, nkb)
    nqb = S // Q_BLOCK
    scale = dh ** -0.5
    gather_bh = jax.vmap(jax.vmap(lambda arr, idx: arr[idx]))
    blk_ids = jnp.arange(nkb)

    def block(i):
        t0 = i * Q_BLOCK
        cur = t0 // B_BLOCK
        qb = lax.dynamic_slice_in_dim(q, t0, Q_BLOCK, 1)
        qpos = t0 + jnp.arange(Q_BLOCK)
        gate = jnp.einsum('bqhd,bnhd->bhqn', qb, kmean).astype(jnp.float32)
        gate = jnp.where(blk_ids < cur, gate, -jnp.inf)
        _, sel = lax.top_k(gate, nsel)
        sel_valid = sel < cur
        kg = gather_bh(kbt, sel)
        vg = gather_bh(vbt, sel)
        s_sel = jnp.einsum('bqhd,bhqnkd->bhqnk', qb, kg).astype(jnp.float32) * scale
        s_sel = jnp.where(sel_valid[..., None], s_sel, -jnp.inf)
        s_sel = s_sel.reshape(Bn, H, Q_BLOCK, nsel * B_BLOCK)
        k_own = lax.dynamic_slice_in_dim(kp, cur * B_BLOCK, B_BLOCK, 1)
        v_own = lax.dynamic_slice_in_dim(vp, cur * B_BLOCK, B_BLOCK, 1)
        own_pos = cur * B_BLOCK + jnp.arange(B_BLOCK)
        s_own = jnp.einsum('bqhd,bkhd->bhqk', qb, k_own).astype(jnp.float32) * scale
        s_own = jnp.where((own_pos[None, :] <= qpos[:, None])[None, None], s_own, -jnp.inf)
        p = jax.nn.softmax(jnp.concatenate([s_own, s_sel], axis=-1), axis=-1).astype(v.dtype)
        p_own = p[..., :B_BLOCK]
        p_sel = p[..., B_BLOCK:].reshape(Bn, H, Q_BLOCK, nsel, B_BLOCK)
        return (jnp.einsum('bhqk,bkhd->bqhd', p_own, v_own)
                + jnp.einsum('bhqnk,bhqnkd->bqhd', p_sel, vg))

    out = lax.map(block, jnp.arange(nqb))
    return out.transpose(1, 0, 2, 3, 4).reshape(Bn, S, H * dh)


def swa_sink_attention(q, k, v, sinks):
    Bn, S, HC, dh = q.shape
    KV = k.shape[2]
    G = HC // KV
    W = C_WINDOW
    nb = S // W
    scale = dh ** -0.5
    qb = q.reshape(Bn, nb, W, KV, G, dh)
    kb = k.reshape(Bn, nb, W, KV, dh)
    vb = v.reshape(Bn, nb, W, KV, dh)
    kband = jnp.concatenate([jnp.pad(kb, ((0, 0), (1, 0), (0, 0), (0, 0), (0, 0)))[:, :-1], kb], axis=2)
    vband = jnp.concatenate([jnp.pad(vb, ((0, 0), (1, 0), (0, 0), (0, 0), (0, 0)))[:, :-1], vb], axis=2)
    s = jnp.einsum('bnqkgd,bnskd->bnkgqs', qb, kband).astype(jnp.float32) * scale
    qrel = jnp.arange(W)[:, None] + W
    krel = jnp.arange(2 * W)[None, :]
    diff = qrel - krel
    kabs = jnp.arange(nb)[:, None, None] * W - W + krel[None]
    mask = (diff >= 0)[None] & (diff < W)[None] & (kabs >= 0)
    s = jnp.where(mask[None, :, None, None], s, -jnp.inf)
    sink = jnp.broadcast_to(sinks.astype(jnp.float32).reshape(1, 1, KV, G, 1, 1), s.shape[:-1] + (1,))
    p = jax.nn.softmax(jnp.concatenate([s, sink], axis=-1), axis=-1)[..., :-1].astype(v.dtype)
    out = jnp.einsum('bnkgqs,bnskd->bnqkgd', p, vband)
    return out.reshape(Bn, S, HC * dh)


def mixer(x, w_in, a_w_uk, a_w_uv, c_sinks, w_branch, w_o, cos, sin):
    Bn, S, _ = x.shape
    h = x @ w_in
    (a_q, a_c, a_iq, a_ik, a_iw, b_q, b_k, b_v, c_q, c_k, c_v, gates) = jnp.split(
        h, _split_points(IN_SIZES), axis=-1)
    heads = lambda t, n: t.reshape(Bn, S, n, -1)
    qa = apply_rope(heads(a_q, A_HEADS), cos, sin)
    ka = apply_rope((a_c @ a_w_uk)[:, :, None, :], cos, sin)[:, :, 0]
    va = a_c @ a_w_uv
    iq = apply_rope(heads(a_iq, A_IDX_HEADS), cos, sin)
    ik = apply_rope(a_ik[:, :, None, :], cos, sin)[:, :, 0]
    o_a = dsa_attention(qa, ka, va, iq, ik, a_iw)
    o_b = moba_attention(apply_rope(heads(b_q, B_HEADS), cos, sin),
                         apply_rope(heads(b_k, B_HEADS), cos, sin),
                         heads(b_v, B_HEADS))
    o_c = swa_sink_attention(apply_rope(heads(c_q, C_HEADS), cos, sin),
                             apply_rope(heads(c_k, C_KV_HEADS), cos, sin),
                             heads(c_v, C_KV_HEADS), c_sinks)
    wa, wb, wc = jnp.split(w_branch, [A_W, A_W + B_W], axis=0)
    ga, gb, gc = jnp.split(jax.nn.sigmoid(gates), N_BRANCH, axis=-1)
    merged = ga * (o_a @ wa) + gb * (o_b @ wb) + gc * (o_c @ wc)
    return merged @ w_o


def moe(x, router_w, router_b, w_gate, w_up, w_down):
    Bn, S, D = x.shape
    xt = x.reshape(-1, D)
    aff = jax.nn.sigmoid((xt @ router_w).astype(jnp.float32))
    sel_score = aff + router_b.astype(jnp.float32)
    grp_score = lax.top_k(sel_score.reshape(-1, N_GROUPS, EXPERTS_PER_GROUP), TOP_K)[0].sum(-1)
    g_star = jnp.argmax(grp_score, axis=-1)
    in_group = (jnp.arange(N_EXPERTS) // EXPERTS_PER_GROUP)[None, :] == g_star[:, None]
    _, top_idx = lax.top_k(jnp.where(in_group, sel_score, -jnp.inf), TOP_K)
    top_aff = jnp.take_along_axis(aff, top_idx, axis=-1)
    wts = top_aff / jnp.sum(top_aff, -1, keepdims=True)
    combine = jnp.sum(jax.nn.one_hot(top_idx, N_EXPERTS, dtype=jnp.float32) * wts[..., None], axis=1)
    combine = combine.astype(x.dtype)
    y = jnp.zeros_like(xt)
    for e in range(N_EXPERTS):
        he = jax.nn.silu(xt @ w_gate[e]) * (xt @ w_up[e])
        y = y + combine[:, e:e + 1] * (he @ w_down[e])
    return y.reshape(Bn, S, D)


def setup_inputs(seed: int = 0) -> dict:
    key = jax.random.key(seed)
    ks = jax.random.split(key, 20)
    nrm = lambda k, shape, scale: jax.random.normal(k, shape, jnp.float32) * scale
    w_branch = jnp.concatenate([
        nrm(ks[5], (DEPTH, A_W, D_MODEL), A_W ** -0.5),
        nrm(ks[6], (DEPTH, B_W, D_MODEL), B_W ** -0.5),
        nrm(ks[7], (DEPTH, C_W, D_MODEL), C_W ** -0.5)], axis=1)
    return {
        "x": nrm(ks[0], (BATCH, SEQ, D_MODEL), 1.0),
        "w_in": nrm(ks[1], (DEPTH, D_MODEL, N_IN), D_MODEL ** -0.5),
        "a_w_uk": nrm(ks[2], (DEPTH, A_KV_RANK, HEAD_DIM), A_KV_RANK ** -0.5),
        "a_w_uv": nrm(ks[3], (DEPTH, A_KV_RANK, HEAD_DIM), A_KV_RANK ** -0.5),
        "c_sinks": nrm(ks[4], (DEPTH, C_HEADS), 1.0),
        "w_branch": w_branch,
        "w_o": nrm(ks[8], (DEPTH, D_MODEL, D_MODEL), D_MODEL ** -0.5 * DN_BETA),
        "ln1_g": 1.0 + nrm(ks[9], (DEPTH, D_MODEL), 0.01),
        "ln1_b": nrm(ks[10], (DEPTH, D_MODEL), 0.01),
        "router_w": nrm(ks[11], (D_MODEL, N_EXPERTS), D_MODEL ** -0.5),
        "router_b": nrm(ks[12], (N_EXPERTS,), 0.01),
        "moe_w_gate": nrm(ks[13], (DEPTH, N_EXPERTS, D_MODEL, D_EXPERT), D_MODEL ** -0.5),
        "moe_w_up": nrm(ks[14], (DEPTH, N_EXPERTS, D_MODEL, D_EXPERT), D_MODEL ** -0.5),
        "moe_w_down": nrm(ks[15], (DEPTH, N_EXPERTS, D_EXPERT, D_MODEL), D_EXPERT ** -0.5 * DN_BETA),
        "ln2_g": 1.0 + nrm(ks[16], (DEPTH, D_MODEL), 0.01),
        "ln2_b": nrm(ks[17], (DEPTH, D_MODEL), 0.01),
    }


def reference(x, w_in, a_w_uk, a_w_uv, c_sinks, w_branch, w_o, ln1_g, ln1_b,
              router_w, router_b, moe_w_gate, moe_w_up, moe_w_down, ln2_g, ln2_b):
    S = x.shape[1]
    cos, sin = rope_tables(S, HEAD_DIM)
    for l in range(DEPTH):
        y = mixer(x, w_in[l], a_w_uk[l], a_w_uv[l], c_sinks[l], w_branch[l], w_o[l], cos, sin)
        x = layer_norm(DN_ALPHA * x + y, ln1_g[l], ln1_b[l])
        y = moe(x, router_w, router_b, moe_w_gate[l], moe_w_up[l], moe_w_down[l])
        x = layer_norm(DN_ALPHA * x + y, ln2_g[l], ln2_b[l])
    return x
```

```python
import bisect
import math
import numpy as np
import ml_dtypes
import concourse.bass as bass
import concourse.mybir as mybir
from concourse.bass_utils import run_bass_kernel_spmd

F32 = mybir.dt.float32
BF16 = mybir.dt.bfloat16
AF = mybir.ActivationFunctionType
ALU = mybir.AluOpType
AX = mybir.AxisListType

NCORES = 4
S_LEN = 8192
D = 1024
NB = S_LEN // 128
NG = S_LEN // 512
DEPTH = 2
IN_SIZES = (256, 128, 256, 64, 4, 256, 256, 256, 512, 128, 128, 3072)
N_IN = sum(IN_SIZES)
NFM = 53
NTM = 388
DN_ALPHA = (2 * DEPTH) ** 0.25
LN_EPS = 1e-5
NEG = -30000.0
BIG = 1.0e30
TOPK = 256
NBIS = 18
NE = 16
DE = 512


class Op:
    __slots__ = ("eng", "fn", "waits", "sig", "idx", "dma_sem")

    def __init__(self, eng, fn):
        self.eng = eng
        self.fn = fn
        self.waits = []
        self.sig = None
        self.idx = None
        self.dma_sem = None


class Buf:
    __slots__ = ("name", "lw", "rd", "excl")

    def __init__(self, name="", excl=False):
        self.name = name
        self.lw = None
        self.rd = []
        self.excl = excl


class T:
    __slots__ = ("t", "b")

    def __init__(self, t, b):
        self.t = t
        self.b = b


class Ring:
    def __init__(self, items):
        self.items = items
        self.i = 0

    def next(self):
        it = self.items[self.i % len(self.items)]
        self.i += 1
        return it


class Sched:
    ENGS = ("pe", "act", "dve", "pool", "sp")

    def __init__(self, nc, n_dma_sems=16):
        self.nc = nc
        self.ops = {e: [] for e in self.ENGS}
        self.count = {e: 0 for e in self.ENGS}
        self.sig_idx = {e: [] for e in self.ENGS}
        self.known = {e: {} for e in self.ENGS}
        self.n_dma_sems = n_dma_sems
        self.dma_cnt = {}
        self.dma_rr = {e: 0 for e in self.ENGS}
        self.dma_last = {}
        self.last_real = {e: None for e in self.ENGS}

    def ticket(self, op):
        if op.dma_sem is not None:
            return op.dma_sem
        e = op.eng
        if op.sig is not None:
            return (e, op.sig)
        lst = self.sig_idx[e]
        j = bisect.bisect_left(lst, op.idx)
        if j < len(lst):
            return (e, self.ops[e][lst[j]].sig)
        self.count[e] += 1
        op.sig = self.count[e]
        lst.append(op.idx)
        return (e, op.sig)

    def need(self, op, dep):
        if dep is None:
            return
        if dep.eng == "pe" and op.eng == "pe" and dep.dma_sem is None and op.dma_sem is None:
            return
        import os as _o
        if _o.environ.get("NOSELF") == "1" and dep.eng == op.eng and dep.dma_sem is None and op.dma_sem is None:
            return
        k, v = self.ticket(dep)
        kn = self.known[op.eng]
        if kn.get(k, 0) >= v:
            return
        kn[k] = v
        op.waits.append((k, v))

    def add(self, eng, fn, reads=(), writes=(), dma=False):
        op = Op(eng, fn)
        op.idx = len(self.ops[eng])
        if dma:
            op.dma_sem = ("pending", 0)
        for b in reads:
            self.need(op, b.lw)
            if b.excl:
                last = {}
                for r in b.rd:
                    if r.eng != eng and (r.eng not in last or r.idx > last[r.eng].idx):
                        last[r.eng] = r
                for r in last.values():
                    self.need(op, r)
        for b in writes:
            self.need(op, b.lw)
            last = {}
            for r in b.rd:
                if r.dma_sem is not None:
                    self.need(op, r)
                elif r.eng not in last or r.idx > last[r.eng].idx:
                    last[r.eng] = r
            for r in last.values():
                self.need(op, r)
        if dma:
            j = self.dma_rr[eng]
            self.dma_rr[eng] = (j + 1) % self.n_dma_sems
            key = ("dma", eng, j)
            prev = self.dma_last.get(key)
            if prev is not None:
                self.need(op, prev)
            self.dma_cnt[key] = self.dma_cnt.get(key, 0) + 1
            op.dma_sem = (key, 16 * self.dma_cnt[key])
            self.dma_last[key] = op
        self.ops[eng].append(op)
        self.last_real[eng] = op
        for b in writes:
            b.lw = op
            b.rd = []
        for b in reads:
            b.rd.append(op)
        return op

    def barrier(self):
        deps = [o for o in self.last_real.values() if o is not None and o.dma_sem is None]
        deps += list(self.dma_last.values())
        for e in self.ENGS:
            op = Op(e, None)
            op.idx = len(self.ops[e])
            for d in deps:
                if d.eng == e and d.dma_sem is None:
                    continue
                self.need(op, d)
            self.ops[e].append(op)

    def emit(self, final_ops):
        nc = self.nc
        fin = Op("sp", None)
        fin.idx = len(self.ops["sp"])
        for o in final_ops:
            self.need(fin, o)
        self.ops["sp"].append(fin)
        semkeys = [e for e in self.ENGS if self.count[e] > 0] + list(self.dma_cnt.keys())
        sems = {}
        for i, k in enumerate(semkeys):
            sems[k] = nc.alloc_semaphore(name=f"sem{i}")
        with nc.allow_low_precision(reason="bf16 matmul operands, fp32 accumulation"), nc.Block() as block:
            def mk(e):
                def body(h):
                    for op in self.ops[e]:
                        for (k, v) in op.waits:
                            h.wait_ge(sems[k], v)
                        if op.fn is None:
                            continue
                        inst = op.fn(h)
                        if op.dma_sem is not None:
                            inst.then_inc(sems[op.dma_sem[0]], 16)
                        elif op.sig is not None:
                            inst.then_inc(sems[e], 1)
                return body
            block.tensor(mk("pe"))
            block.scalar(mk("act"))
            block.vector(mk("dve"))
            block.gpsimd(mk("pool"))
            block.sync(mk("sp"))


class Alloc:
    def __init__(self, nc, base, top):
        self.nc = nc
        self.cur = base
        self.top = top
        self.n = 0

    def mark(self):
        return self.cur

    def reset(self, m):
        self.cur = m

    def tile(self, name, shape, dtype, nbuf=None):
        size = 2 if dtype == BF16 else 4
        nbytes = size
        for s in shape[1:]:
            nbytes *= s
        off = (self.cur + 63) // 64 * 64
        assert off + nbytes <= self.top, f"SBUF overflow allocating {name}: {off}+{nbytes} > {self.top}"
        self.n += 1
        t = self.nc.alloc_sbuf_tensor_at(f"{name}_{self.n}", list(shape), dtype, offset=off)
        self.cur = off + nbytes
        return T(t, Buf(name))

    def ring(self, name, shape, dtype, n):
        return Ring([self.tile(f"{name}{i}", shape, dtype) for i in range(n)])


def build_program(debug=False, stages=("P", "A1", "A2", "M"), depth=DEPTH):
    nc = bass.Bass("TRN2", target_bir_lowering=False)
    import os as _os
    S = Sched(nc, n_dma_sems=int(_os.environ.get("NDS", "16")))

    def din(name, shape, dt=F32):
        return nc.dram_tensor(name, list(shape), dt, kind="ExternalInput").ap()

    scr_kind = "ExternalOutput" if debug else "Internal"

    def dscr(name, shape, dt):
        return nc.dram_tensor(name, list(shape), dt, kind=scr_kind).ap()

    x_d = din("x", [S_LEN, D])
    wfm_d = din("w_fm", [DEPTH, NFM, 128, 8, 128])
    wtm_d = din("w_tm", [DEPTH, 128, 8, NTM])
    uk_d = din("uk", [DEPTH, 128, 64])
    uksw_d = din("uksw", [DEPTH, 128, 64])
    uv_d = din("uv", [DEPTH, 128, 64])
    wbr_d = din("w_branch", [DEPTH, D, D])
    wo_d = din("w_o", [DEPTH, D, D])
    lnp_d = din("lnp", [DEPTH, 4, 128, D])
    sinks_d = din("sinks_bc", [DEPTH, 128, 8])
    rw_d = din("rw", [128, 8, NE])
    rb_d = din("rb_bc", [128, NE])
    wg_d = din("moe_w_gate", [DEPTH, NE, D, DE])
    wu_d = din("moe_w_up", [DEPTH, NE, D, DE])
    wd_d = din("moe_w_down", [DEPTH, NE, DE, D])
    cos_d = din("cosT", [128, S_LEN])
    sin_d = din("sinT", [128, S_LEN])
    c_ident_bf = din("c_ident_bf", [128, 128], BF16)
    c_ident_f = din("c_ident_f", [128, 128])
    c_i4 = din("c_i4", [128, 512], BF16)
    c_tri4 = din("c_tri4", [128, 512], BF16)
    c_band4 = din("c_band4", [128, 512], BF16)
    c_trineg = din("c_trineg", [128, 128])
    c_E = din("c_E", [32, 4096], BF16)
    c_pow = din("c_pow", [128, NBIS])
    out_d = nc.dram_tensor("out", [S_LEN, D], F32, kind="ExternalOutput").ap()

    QA_d = dscr("QA_d", [64, 4, S_LEN], BF16)
    IQ_d = dscr("IQ_d", [64, 4, S_LEN], BF16)
    IK_d = dscr("IK_d", [64, S_LEN], BF16)
    KA_d = dscr("KA_d", [64, S_LEN], BF16)
    QB_d = dscr("QB_d", [128, 2, S_LEN], BF16)
    KB_d = dscr("KB_d", [128, 2, S_LEN], BF16)
    QC_d = dscr("QC_d", [128, 4, S_LEN], BF16)
    KC_d = dscr("KC_d", [128, S_LEN], BF16)
    GT_d = dscr("GT_d", [128, 24, S_LEN], BF16)
    VA_d = dscr("VA_d", [128, NB, 65], BF16)
    VB_d = dscr("VB_d", [128, NB, 260], BF16)
    VC_d = dscr("VC_d", [S_LEN, 130], BF16)
    IW_d = dscr("IW_d", [S_LEN, 8], F32)
    OAB_d = dscr("OAB_d", [S_LEN, 512], BF16)
    X1_d = dscr("X1_d", [S_LEN, D], F32)
    XL_d = dscr("XL_d", [S_LEN, D], F32)
    if debug:
        DBGo_d = dscr("DBGo_d", [S_LEN, D], BF16)
        DBGm_d = dscr("DBGm_d", [128, 8, S_LEN], BF16)
        DBGr_d = dscr("DBGr_d", [S_LEN, D], F32)
    DB = {}

    def db(name, i):
        k = (name, i)
        if k not in DB:
            DB[k] = Buf(f"{name}{i}")
        return DB[k]

    def dbg_all(name):
        return [db(name, g) for g in range(NG)]

    def MM(out, lhsT, rhs, start, stop, R, W, skip=False):
        S.add("pe", lambda h: h.matmul(out, lhsT=lhsT, rhs=rhs, start=start, stop=stop, skip_group_check=skip), reads=R, writes=W)

    def TR(out, in_, ident, R, W):
        S.add("pe", lambda h: h.transpose(out=out, in_=in_, identity=ident), reads=R, writes=W)

    def ACT(out, in_, func, R, W, scale=1.0, bias=None, accum=None):
        if bias is None:
            S.add("act", lambda h: h.activation(out=out, in_=in_, func=func, scale=scale), reads=R, writes=W)
        else:
            S.add("act", lambda h: h.activation(out=out, in_=in_, func=func, scale=scale, bias=bias), reads=R, writes=W)

    def TS(eng, out, in0, s1, op0, R, W, s2=None, op1=None, accum=None):
        def f(h):
            kw = {}
            if op1 is not None:
                kw["op1"] = op1
            if accum is not None:
                kw["accum_out"] = accum
            return h.tensor_scalar(out=out, in0=in0, scalar1=s1, scalar2=s2, op0=op0, **kw)
        S.add(eng, f, reads=R, writes=W)

    def TT(eng, out, in0, in1, op, R, W):
        S.add(eng, lambda h: h.tensor_tensor(out=out, in0=in0, in1=in1, op=op), reads=R, writes=W)

    def STT(out, in0, scalar, in1, op0, op1, R, W):
        S.add("dve", lambda h: h.scalar_tensor_tensor(out=out, in0=in0, scalar=scalar, in1=in1, op0=op0, op1=op1), reads=R, writes=W)

    def RED(out, in_, op, R, W, axis=AX.X):
        S.add("dve", lambda h: h.tensor_reduce(out=out, in_=in_, axis=axis, op=op), reads=R, writes=W)

    def CP(eng, out, in_, R, W):
        if eng == "act":
            S.add("act", lambda h: h.activation(out=out, in_=in_, func=AF.Copy), reads=R, writes=W)
        else:
            S.add(eng, lambda h: h.tensor_copy(out=out, in_=in_), reads=R, writes=W)

    def RECIP(out, in_, R, W):
        S.add("dve", lambda h: h.reciprocal(out=out, in_=in_), reads=R, writes=W)

    def MAX8(out, in_, R, W):
        S.add("dve", lambda h: h.max(out=out, in_=in_), reads=R, writes=W)

    def MSET(eng, ap, val, W):
        S.add(eng, lambda h: h.memset(ap, val), writes=W)

    def DMA(eng, out, in_, R, W, mdl=4096):
        if eng == "pool":
            return S.add(eng, lambda h: h.dma_start(out=out, in_=in_, max_dma_last_dim=mdl), reads=R, writes=W, dma=True)
        return S.add(eng, lambda h: h.dma_start(out=out, in_=in_), reads=R, writes=W, dma=True)

    A = Alloc(nc, 16512, 229344)
    PSB = [T(nc.alloc_psum_tensor(f"psb{i}", [128, 512], F32), Buf(f"psb{i}", excl=True)) for i in range(8)]
    PS = Ring(PSB[0:6])
    PO = Ring(PSB[6:8])

    ident_bf = A.tile("ident_bf", [128, 128], BF16)
    ident_f = A.tile("ident_f", [128, 128], F32)
    i4 = A.tile("i4", [128, 512], BF16)
    tri4 = A.tile("tri4", [128, 512], BF16)
    band4 = A.tile("band4", [128, 512], BF16)
    trineg = A.tile("trineg", [128, 128], F32)
    Ec = A.tile("Ec", [32, 4096], BF16)
    cpow = A.tile("cpow", [128, NBIS], F32)
    neg1 = A.tile("neg1", [128, 1], F32)
    rw = A.tile("rw", [128, 8, NE], F32)
    rb = A.tile("rb", [128, NE], F32)
    for tl, src in ((ident_bf, c_ident_bf), (ident_f, c_ident_f), (i4, c_i4), (tri4, c_tri4), (band4, c_band4),
                    (trineg, c_trineg), (Ec, c_E), (cpow, c_pow), (rw, rw_d), (rb, rb_d)):
        DMA("sp", tl.t[:], src, [], [tl.b])
    MSET("dve", neg1.t[:], -1.0, [neg1.b])
    base_mark = A.mark()

    out_ops = []

    def layer_norm(r, g_bc, b_bc, xo, tmp):
        st, mv, sd = tmp["st"], tmp["mv"], tmp["sd"]
        S.add("dve", lambda h: h.bn_stats(out=st.t[:, 0, :], in_=r.t[:, 0:512]), reads=[r.b], writes=[st.b])
        S.add("dve", lambda h: h.bn_stats(out=st.t[:, 1, :], in_=r.t[:, 512:1024]), reads=[r.b], writes=[st.b])
        S.add("dve", lambda h: h.bn_aggr(out=mv.t[:], in_=st.t[:]), reads=[st.b], writes=[mv.b])
        TS("dve", sd.t[:, 0:1], mv.t[:, 1:2], LN_EPS, ALU.add, [mv.b], [sd.b])
        ACT(sd.t[:, 1:2], sd.t[:, 0:1], AF.Sqrt, [sd.b], [sd.b])
        S.add("dve", lambda h: h.reciprocal(out=sd.t[:, 2:3], in_=sd.t[:, 1:2]), reads=[sd.b], writes=[sd.b])
        TS("dve", r.t[:], r.t[:], mv.t[:, 0:1], ALU.subtract, [r.b, mv.b, sd.b], [r.b], s2=sd.t[:, 2:3], op1=ALU.mult)
        TT("dve", r.t[:], r.t[:], g_bc.t[:], ALU.mult, [r.b, g_bc.b], [r.b])
        TT("dve", xo.t[:], r.t[:], b_bc.t[:], ALU.add, [r.b, b_bc.b], [xo.b])

    for l in range(depth):
        xin_d = x_d if l == 0 else XL_d
        xin_name = "x" if l == 0 else "XL"

        if "P" in stages:
            A.reset(base_mark)
            Wfm = [A.tile(f"wfm{ci}", [128, 8, 128], BF16) for ci in range(NFM)]
            Wtm = A.tile("wtm", [128, 8, NTM], BF16)
            ukt = A.tile("uk", [128, 64], BF16)
            ukswt = A.tile("uksw", [128, 64], BF16)
            uvt = A.tile("uv", [128, 64], BF16)
            xf_r = A.ring("xf", [128, D], F32, 8)
            xb_r = A.ring("xb", [128, D], BF16, 2)
            xT_r = A.ring("xT", [128, 8, 512], BF16, 2)
            cs_r = A.ring("cs", [128, 512], F32, 2)
            sn_r = A.ring("sn", [128, 512], F32, 2)
            t1_r = A.ring("t1", [128, 512], F32, 2)
            t2_r = A.ring("t2", [128, 512], F32, 2)
            ost_r = A.ring("ost", [128, 512], BF16, 4)
            acT_r = A.ring("acT", [128, 512], BF16, 2)
            vast_r = A.ring("vast", [128, 65], BF16, 2)
            vbst_r = A.ring("vbst", [128, 4, 65], BF16, 2)
            vcst_r = A.ring("vcst", [128, 2, 65], BF16, 2)
            iwst_r = A.ring("iwst", [128, 8], F32, 2)
            for rg in (vast_r, vbst_r, vcst_r):
                for tl in rg.items:
                    MSET("dve", tl.t[:], 1.0, [tl.b])
            order = list(range(NFM))
            for ci in order:
                DMA("pool", Wfm[ci].t[:], wfm_d[l, ci], [], [Wfm[ci].b])
            DMA("pool", Wtm.t[:], wtm_d[l], [], [Wtm.b], mdl=NTM * 4)
            DMA("pool", ukt.t[:], uk_d[l], [], [ukt.b])
            DMA("pool", ukswt.t[:], uksw_d[l], [], [ukswt.b])
            DMA("pool", uvt.t[:], uv_d[l], [], [uvt.b])

            def load_group(g):
                res = []
                for tb in range(4):
                    blk = g * 4 + tb
                    xf = xf_r.next()
                    DMA("sp", xf.t[:], xin_d[blk * 128:(blk + 1) * 128, :], [db(xin_name, blk)], [xf.b])
                    res.append(xf)
                cs = cs_r.next()
                sn = sn_r.next()
                DMA("sp", cs.t[:], cos_d[:, g * 512:(g + 1) * 512], [], [cs.b])
                DMA("sp", sn.t[:], sin_d[:, g * 512:(g + 1) * 512], [], [sn.b])
                return res, cs, sn

            def rope_out(pa, pb_, cs, sn, np_):
                t1 = t1_r.next()
                t2 = t2_r.next()
                ost = ost_r.next()
                TT("dve", t1.t[0:np_, :], pa.t[0:np_, :], cs.t[0:np_, :], ALU.mult, [pa.b, cs.b], [t1.b])
                TT("dve", t2.t[0:np_, :], pb_.t[0:np_, :], sn.t[0:np_, :], ALU.mult, [pb_.b, sn.b], [t2.b])
                TT("dve", ost.t[0:np_, :], t1.t[0:np_, :], t2.t[0:np_, :], ALU.add, [t1.b, t2.b], [ost.b])
                return ost

            def unit_dst(u, ts):
                if u in (0, 1):
                    return [(QA_d[:, 2 * u, ts], 0, 64), (QA_d[:, 2 * u + 1, ts], 64, 128)], "QA"
                if u in (2, 3):
                    c = u - 2
                    return [(IQ_d[:, 2 * c, ts], 0, 64), (IQ_d[:, 2 * c + 1, ts], 64, 128)], "IQ"
                if u == 4:
                    return [(IK_d[:, ts], 0, 64)], "IK"
                if u in (5, 6):
                    return [(QB_d[:, u - 5, ts], 0, 128)], "QB"
                if u in (7, 8):
                    return [(KB_d[:, u - 7, ts], 0, 128)], "KB"
                if u in (9, 10, 11, 12):
                    c = u - 9
                    res = []
                    for half in range(2):
                        hh = 2 * c + half
                        gq, r = hh // 4, hh % 4
                        res.append((QC_d[gq * 64:(gq + 1) * 64, r, ts], half * 64, half * 64 + 64))
                    return res, "QC"
                return [(KC_d[:, ts], 0, 128)], "KC"

            import os
            PD = int(os.environ.get("PDBG", "15"))
            NGR = int(os.environ.get("PNG", str(NG)))
            nxt = load_group(0)
            for g in range(NGR):
                xfs, cs, sn = nxt
                if g + 1 < NGR:
                    nxt = load_group(g + 1)
                ts = slice(g * 512, (g + 1) * 512)
                xT = xT_r.next()
                for tb in range(4):
                    xb = xb_r.next()
                    CP("dve" if tb % 2 == 0 else "act", xb.t[:], xfs[tb].t[:], [xfs[tb].b], [xb.b])
                    pt = PS.next()
                    ptv = pt.t[:].bitcast(BF16).rearrange("p (k q) -> p k q", k=8)
                    for kc in range(8):
                        TR(ptv[:, kc, :], xb.t[:, kc * 128:(kc + 1) * 128], ident_bf.t[:], [xb.b, ident_bf.b], [pt.b])
                    CP("act" if tb % 2 == 0 else "dve", xT.t[:, :, tb * 128:(tb + 1) * 128], ptv, [pt.b], [xT.b])
                ulist = [int(v) for v in os.environ.get("PUNITS", ",".join(str(v) for v in range(14))).split(",")]
                for u in (ulist if PD & 1 else []):
                    pa = PS.next()
                    pb_ = PS.next()
                    for kc in range(8):
                        MM(pa.t[:], Wfm[2 * u].t[:, kc, :], xT.t[:, kc, :], kc == 0, kc == 7, [Wfm[2 * u].b, xT.b], [pa.b])
                    for kc in range(8):
                        MM(pb_.t[:], Wfm[2 * u + 1].t[:, kc, :], xT.t[:, kc, :], kc == 0, kc == 7, [Wfm[2 * u + 1].b, xT.b], [pb_.b])
                    dsts, nm = unit_dst(u, ts)
                    np_ = 64 if u == 4 else 128
                    ost = rope_out(pa, pb_, cs, sn, np_)
                    for (dap, p0, p1) in dsts:
                        DMA("sp", dap, ost.t[p0:p1, :], [ost.b], [db(nm, g)])
                if not (PD & 2):
                    continue
                pa = PS.next()
                for kc in range(8):
                    MM(pa.t[:], Wfm[28].t[:, kc, :], xT.t[:, kc, :], kc == 0, kc == 7, [Wfm[28].b, xT.b], [pa.b])
                acT = acT_r.next()
                CP("act", acT.t[:], pa.t[:], [pa.b], [acT.b])
                pa = PS.next()
                pb_ = PS.next()
                MM(pa.t[0:64, :], ukt.t[:], acT.t[:], True, True, [ukt.b, acT.b], [pa.b])
                MM(pb_.t[0:64, :], ukswt.t[:], acT.t[:], True, True, [ukswt.b, acT.b], [pb_.b])
                ost = rope_out(pa, pb_, cs, sn, 64)
                DMA("sp", KA_d[:, ts], ost.t[0:64, :], [ost.b], [db("KA", g)])
                for tb in range(4):
                    blk = g * 4 + tb
                    pv = PS.next()
                    MM(pv.t[:, 0:64], acT.t[:, tb * 128:(tb + 1) * 128], uvt.t[:], True, True, [acT.b, uvt.b], [pv.b])
                    vast = vast_r.next()
                    CP("act", vast.t[:, 0:64], pv.t[:, 0:64], [pv.b], [vast.b])
                    DMA("sp", VA_d[:, blk, :], vast.t[:], [vast.b], [db("VA", g)])
                for j in range(24 if PD & 4 else 0):
                    pa = PS.next()
                    W_ = Wfm[29 + j]
                    for kc in range(8):
                        MM(pa.t[:], W_.t[:, kc, :], xT.t[:, kc, :], kc == 0, kc == 7, [W_.b, xT.b], [pa.b])
                    ost = ost_r.next()
                    ACT(ost.t[:], pa.t[:], AF.Sigmoid, [pa.b], [ost.b])
                    DMA("sp", GT_d[:, j, ts], ost.t[:], [ost.b], [db("GT", g)])
                for tb in range(4 if PD & 8 else 0):
                    blk = g * 4 + tb
                    pv = PS.next()
                    for kc in range(8):
                        MM(pv.t[:, 0:NTM], xT.t[:, kc, tb * 128:(tb + 1) * 128], Wtm.t[:, kc, :], kc == 0, kc == 7, [xT.b, Wtm.b], [pv.b])
                    vbst = vbst_r.next()
                    vcst = vcst_r.next()
                    iwst = iwst_r.next()
                    TMO = int(os.environ.get("TMO", "7"))
                    if TMO & 1:
                        CP("act", vbst.t[:, :, 0:64], pv.t[:, 0:256].rearrange("p (h d) -> p h d", h=4), [pv.b], [vbst.b])
                    if TMO & 2:
                        CP("dve", vcst.t[:, :, 0:64], pv.t[:, 256:384].rearrange("p (h d) -> p h d", h=2), [pv.b], [vcst.b])
                    if TMO & 4:
                        CP("dve", iwst.t[:, 4:8], pv.t[:, 384:388], [pv.b], [iwst.b])
                        STT(iwst.t[:, 0:4], pv.t[:, 384:388], -1.0, iwst.t[:, 4:8], ALU.mult, ALU.max, [pv.b, iwst.b], [iwst.b])
                    TMD = int(os.environ.get("TMD", "15"))
                    if TMD & 1:
                        ACT(iwst.t[:, 4:8], pv.t[:, 384:388], AF.Sign, [pv.b], [iwst.b])
                    if TMD & 2:
                        DMA("sp", VB_d[:, blk, :], vbst.t[:].rearrange("p h d -> p (h d)"), [vbst.b], [db("VB", g)])
                    if TMD & 4:
                        DMA("sp", VC_d[blk * 128:(blk + 1) * 128, :], vcst.t[:].rearrange("p h d -> p (h d)"), [vcst.b], [db("VC", blk)])
                    if TMD & 8:
                        DMA("sp", IW_d[blk * 128:(blk + 1) * 128, :], iwst.t[:], [iwst.b], [db("IW", blk)])
            S.barrier()

        if "A1" in stages:
            A.reset(base_mark)
            KI = A.tile("KI", [128, S_LEN], BF16)
            ka_b, ik_b = Buf("ka"), Buf("ik")
            VA = A.tile("VA", [128, NB, 65], BF16)
            KBt = A.tile("KB", [128, 2, S_LEN], BF16)
            VBt = A.tile("VB", [128, NB, 260], BF16)
            acc = A.tile("acc", [128, S_LEN], F32)
            mb = A.tile("mb", [128, S_LEN], BF16)
            kms = A.tile("kms", [128, 2, 32], F32)
            kmT = A.tile("kmT", [128, 2, 32], BF16)
            gs = A.tile("gs", [128, 4, 32], F32)
            m8 = A.tile("m8", [128, 4, 8], F32)
            mbm = A.tile("mbm", [128, 4, 32], BF16)
            mbT = A.tile("mbT", [32, 512], BF16)
            QI_r = A.ring("QI", [128, 4, 128], BF16, 2)
            QA_r = A.ring("QAz", [128, 4, 128], BF16, 2)
            QB_r = A.ring("QB", [128, 4, 128], BF16, 2)
            for rg in (QI_r, QA_r, QB_r):
                for tl in rg.items:
                    MSET("dve", tl.t[:], 0.0, [tl.b])
            IW_r = A.ring("IW", [128, 8], F32, 2)
            pT_r = A.ring("pT", [128, 512], BF16, 4)
            oab_r = A.ring("oab", [128, 512], BF16, 2)
            bs = A.tile("bs", [128, 8], F32)
            HW = A.tile("HW", [128, NBIS], F32)
            rec = A.tile("rec", [128, 8], F32)

            DMA("sp", KI.t[0:64, :], KA_d, dbg_all("KA"), [ka_b])
            DMA("sp", KI.t[64:128, :], IK_d, dbg_all("IK"), [ik_b])
            DMA("sp", VA.t[:], VA_d, dbg_all("VA"), [VA.b])
            for c in range(2):
                DMA("sp", KBt.t[:, c, :], KB_d[:, c, :], dbg_all("KB"), [KBt.b])
            for c in range(4):
                DMA("sp", VBt.t[:, c * 16:(c + 1) * 16, :], VB_d[:, c * 16:(c + 1) * 16, :], dbg_all("VB"), [VBt.b])
            MSET("dve", gs.t[:], -BIG, [gs.b])
            for c in range(2):
                RED(kms.t[:, c, :], KBt.t[:, c, :].rearrange("p (n k) -> p n k", k=256), ALU.add, [KBt.b], [kms.b])
            TS("dve", kmT.t[:], kms.t[:], 1.0 / 256.0, ALU.mult, [kms.b], [kmT.b])

            def load_a1(i):
                QI = QI_r.next()
                QAz = QA_r.next()
                QB = QB_r.next()
                IW = IW_r.next()
                bsl = slice(i * 128, (i + 1) * 128)
                g = i // 4
                DMA("sp", QAz.t[0:64, :, :], QA_d[:, :, bsl], [db("QA", g)], [QAz.b])
                DMA("sp", QI.t[64:128, :, :], IQ_d[:, :, bsl], [db("IQ", g)], [QI.b])
                for h in range(4):
                    p0 = (h % 2) * 64
                    DMA("sp", QB.t[p0:p0 + 64, h, :], QB_d[p0:p0 + 64, h // 2, bsl], [db("QB", g)], [QB.b])
                DMA("sp", IW.t[:], IW_d[bsl, :], [db("IW", i)], [IW.b])
                return QI, QAz, QB, IW

            import os
            NB1 = int(os.environ.get("A1NB", str(NB)))
            AP_ = int(os.environ.get("A1PARTS", "31"))
            nxt = load_a1(0)
            for i in range(NB1):
                QI, QAz, QB, IW = nxt
                if i + 1 < NB1:
                    nxt = load_a1(i + 1)
                nk = 128 * (i + 1)
                cur = i // 2
                if cur > 0 and (AP_ & 1):
                    pg = PS.next()
                    for h in range(4):
                        p0 = (h % 2) * 64
                        MM(pg.t[:, h * 32:(h + 1) * 32], QB.t[:, h, :], kmT.t[:, h // 2, :],
                           h == 0, h == 3, [QB.b, kmT.b], [pg.b], skip=True)
                    CP("dve", gs.t[:, :, 0:cur], pg.t[:, 0:128].rearrange("p (h n) -> p h n", h=4)[:, :, 0:cur], [pg.b], [gs.b])
                    for h in range(4):
                        MAX8(m8.t[:, h, :], gs.t[:, h, :], [gs.b], [m8.b])
                    for h in range(4):
                        TS("dve", mbm.t[:, h, :], gs.t[:, h, :], m8.t[:, h, 2:3], ALU.is_lt, [gs.b, m8.b], [mbm.b], s2=NEG, op1=ALU.mult)
                    pt = PS.next()
                    ptv = pt.t[:].bitcast(BF16)
                    for h in range(4):
                        TR(ptv[0:32, h * 128:(h + 1) * 128], mbm.t[:, h, :], ident_bf.t[:], [mbm.b, ident_bf.b], [pt.b])
                    CP("act", mbT.t[:], ptv[0:32, 0:512], [pt.b], [mbT.b])
                if i >= 2 and (AP_ & 2):
                    ntile = (nk + 511) // 512
                    for j in range(ntile):
                        w = min(512, nk - 512 * j)
                        ks = slice(j * 512, j * 512 + w)
                        for h in range(4):
                            pi = PS.next()
                            MM(pi.t[:, 0:w], QI.t[:, h, :], KI.t[:, ks], True, True, [QI.b, ik_b, ka_b], [pi.b])
                            ACT(pi.t[:, 0:w], pi.t[:, 0:w], AF.Relu, [pi.b, IW.b], [pi.b], scale=IW.t[:, h:h + 1])
                            if h == 0:
                                TS("dve", acc.t[:, ks], pi.t[:, 0:w], IW.t[:, 4:5], ALU.mult, [pi.b, IW.b], [acc.b])
                            else:
                                STT(acc.t[:, ks], pi.t[:, 0:w], IW.t[:, 4 + h:5 + h], acc.t[:, ks], ALU.mult, ALU.add, [pi.b, IW.b, acc.b], [acc.b])
                    dsl = slice(i * 128, (i + 1) * 128)
                    TT("dve", acc.t[:, dsl], acc.t[:, dsl], trineg.t[:], ALU.add, [acc.b, trineg.b], [acc.b])
                    RED(bs.t[:, 5:6], acc.t[:, 0:nk], ALU.max, [acc.b], [bs.b])
                    RED(bs.t[:, 0:1], acc.t[:, 0:i * 128], ALU.min, [acc.b], [bs.b])
                    TT("dve", bs.t[:, 1:2], bs.t[:, 5:6], bs.t[:, 0:1], ALU.subtract, [bs.b], [bs.b])
                    TS("dve", HW.t[:], cpow.t[:], bs.t[:, 1:2], ALU.mult, [cpow.b, bs.b], [HW.b])
                    for k in range(NBIS):
                        TT("dve", bs.t[:, 2:3], bs.t[:, 0:1], HW.t[:, k:k + 1], ALU.add, [bs.b, HW.b], [bs.b])
                        TS("dve", mb.t[:, 0:nk], acc.t[:, 0:nk], bs.t[:, 2:3], ALU.is_ge, [acc.b, bs.b], [mb.b, bs.b],
                           s2=None, op1=ALU.add, accum=bs.t[:, 3:4])
                        TS("dve", bs.t[:, 4:5], bs.t[:, 3:4], TOPK - 0.5, ALU.is_ge, [bs.b], [bs.b])
                        STT(bs.t[:, 0:1], HW.t[:, k:k + 1], bs.t[:, 4:5], bs.t[:, 0:1], ALU.mult, ALU.add, [bs.b, HW.b], [bs.b])
                    thr = bs.t[:, 0:1]
                    thr_b = bs.b
                else:
                    MSET("dve", acc.t[:, 0:nk], 0.0, [acc.b])
                    dsl = slice(i * 128, (i + 1) * 128)
                    TT("dve", acc.t[:, dsl], acc.t[:, dsl], trineg.t[:], ALU.add, [acc.b, trineg.b], [acc.b])
                    thr = neg1.t[:, 0:1]
                    thr_b = neg1.b
                TS("dve", mb.t[:, 0:nk], acc.t[:, 0:nk], thr, ALU.is_lt, [acc.b, thr_b], [mb.b], s2=NEG, op1=ALU.mult)
                OB = PO.next()
                for kb in range(i + 1 if AP_ & 4 else 0):
                    ks = slice(kb * 128, (kb + 1) * 128)
                    pst = PS.next()
                    first = True
                    if kb < 2 * cur:
                        n = kb // 2
                        MM(pst.t[:], Ec.t[:, n * 128:(n + 1) * 128], mbT.t[:], True, False, [Ec.b, mbT.b], [pst.b], skip=True)
                        first = False
                    elif kb == i:
                        MM(pst.t[:], ident_bf.t[:], tri4.t[:], True, False, [ident_bf.b, tri4.b], [pst.b], skip=True)
                        first = False
                    for h in range(4):
                        p0 = (h % 2) * 64
                        MM(pst.t[:, h * 128:(h + 1) * 128], KBt.t[:, h // 2, ks], QB.t[:, h, :],
                           first and h == 0, h == 3, [KBt.b, QB.b], [pst.b], skip=True)
                    pT = pT_r.next()
                    ACT(pT.t[:], pst.t[:], AF.Exp, [pst.b], [pT.b], scale=0.125)
                    for h in range(4):
                        MM(OB.t[:, h * 65:(h + 1) * 65], pT.t[:, h * 128:(h + 1) * 128], VBt.t[:, kb, h * 65:(h + 1) * 65],
                           kb == 0 and h == 0, kb == i and h == 3, [pT.b, VBt.b], [OB.b], skip=True)
                OA = PO.next()
                for kb in range(i + 1 if AP_ & 8 else 0):
                    ks = slice(kb * 128, (kb + 1) * 128)
                    pst = PS.next()
                    MM(pst.t[:], mb.t[:, ks], i4.t[:], True, False, [mb.b, i4.b], [pst.b])
                    MM(pst.t[:], KI.t[:, ks], QAz.t[:].rearrange("p h q -> p (h q)"), False, True, [ka_b, ik_b, QAz.b], [pst.b])
                    pT = pT_r.next()
                    ACT(pT.t[:], pst.t[:], AF.Exp, [pst.b], [pT.b], scale=0.125)
                    for h in range(4):
                        MM(OA.t[:, h * 65:(h + 1) * 65], pT.t[:, h * 128:(h + 1) * 128], VA.t[:, kb, :],
                           kb == 0 and h == 0, kb == i and h == 3, [pT.b, VA.b], [OA.b], skip=True)
                oab = oab_r.next()
                for (O_, c0, r0) in (((OA, 0, 0), (OB, 256, 4)) if AP_ & 16 else ()):
                    ov = O_.t[:, 0:260].rearrange("p (h d) -> p h d", d=65)
                    RECIP(rec.t[:, r0:r0 + 4], ov[:, :, 64], [O_.b], [rec.b])
                    for h in range(4):
                        TS("dve", oab.t[:, c0 + h * 64:c0 + (h + 1) * 64], ov[:, h, 0:64], rec.t[:, r0 + h:r0 + h + 1], ALU.mult,
                           [O_.b, rec.b], [oab.b])
                DMA("sp", OAB_d[i * 128:(i + 1) * 128, :], oab.t[:], [oab.b], [db("OAB", i)])
            S.barrier()

        if "A2" in stages:
            A.reset(base_mark)
            wbr = A.tile("wbr", [128, 8, D], BF16)
            wo = A.tile("wo", [128, 8, D], BF16)
            g1 = A.tile("g1", [128, D], F32)
            b1 = A.tile("b1", [128, D], F32)
            esink = A.tile("esink", [128, 8], F32)
            QC_r = A.ring("QC", [128, 2, 4, 128], BF16, 2)
            import os
            for tl in (QC_r.items if not int(os.environ.get("A2SKIP", "0")) & 8 else []):
                MSET("dve", tl.t[:], 0.0, [tl.b])
            KC_r = [A.tile(f"KC{k}", [128, 128], BF16) for k in range(3)]
            VC_r = [A.tile(f"VC{k}", [128, 130], BF16) for k in range(3)]
            gT_r = A.ring("gT", [128, 24, 128], BF16, 2)
            xb_r2 = A.ring("xblk", [128, D], F32, 2)
            oall_r = A.ring("oall", [128, D], BF16, 2)
            oT_r = A.ring("oT", [128, 8, 128], BF16, 2)
            tmp_r = A.ring("mtmp", [128, 3, 128], F32, 2)
            mT_r = A.ring("mT", [128, 8, 128], BF16, 2)
            r_r = A.ring("r", [128, D], F32, 2)
            xo_r = A.ring("xo", [128, D], F32, 2)
            pT_r = A.ring("pT2", [128, 512], BF16, 3)
            rec = A.tile("rec2", [128, 8], F32)
            lnt = {"st": A.tile("st", [128, 2, 6], F32), "mv": A.tile("mv", [128, 2], F32), "sd": A.tile("sd", [128, 4], F32)}
            import os
            SK = int(os.environ.get("A2SKIP", "0"))
            if not SK & 1:
                DMA("pool", wbr.t[:], wbr_d[l].rearrange("(kc p) n -> p kc n", p=128), [], [wbr.b])
                DMA("pool", wo.t[:], wo_d[l].rearrange("(kc p) n -> p kc n", p=128), [], [wo.b])
            if not SK & 2:
                DMA("sp", g1.t[:], lnp_d[l, 0], [], [g1.b])
                DMA("sp", b1.t[:], lnp_d[l, 1], [], [b1.b])
                DMA("sp", esink.t[:], sinks_d[l], [], [esink.b])
            if not SK & 4:
                ACT(esink.t[:], esink.t[:], AF.Exp, [esink.b], [esink.b])

            def load_a2(i):
                bsl = slice(i * 128, (i + 1) * 128)
                g = i // 4
                QC = QC_r.next()
                gT = gT_r.next()
                xblk = xb_r2.next()
                oall = oall_r.next()
                KC = KC_r[i % 3]
                VC = VC_r[i % 3]
                oc_b = Buf("oall_c")
                for gq in range(2):
                    DMA("sp", QC.t[gq * 64:(gq + 1) * 64, gq, :, :], QC_d[gq * 64:(gq + 1) * 64, :, bsl], [db("QC", g)], [QC.b])
                DMA("sp", KC.t[:], KC_d[:, bsl], [db("KC", g)], [KC.b])
                DMA("sp", VC.t[:], VC_d[bsl, :], [db("VC", i)], [VC.b])
                DMA("sp", gT.t[:], GT_d[:, :, bsl], [db("GT", g)], [gT.b])
                DMA("sp", xblk.t[:], xin_d[bsl, :], [db(xin_name, i)], [xblk.b])
                DMA("sp", oall.t[:, 0:512], OAB_d[bsl, :], [db("OAB", i), oc_b], [oall.b])
                return QC, gT, xblk, oall, oc_b

            import os
            NB2 = int(os.environ.get("A2NB", str(NB)))
            nxt = load_a2(0) if NB2 > 0 else None
            for i in range(NB2):
                QC, gT, xblk, oall, oc_b = nxt
                if i + 1 < NB2:
                    nxt = load_a2(i + 1)
                for gq in range(2):
                    OC = PO.next()
                    kbs = ([i - 1] if i > 0 else []) + [i]
                    for kb in kbs:
                        KC = KC_r[kb % 3]
                        VC = VC_r[kb % 3]
                        pst = PS.next()
                        mk = band4 if kb == i - 1 else tri4
                        MM(pst.t[:], ident_bf.t[:], mk.t[:], True, False, [ident_bf.b, mk.b], [pst.b])
                        MM(pst.t[:], KC.t[:, :], QC.t[:, gq, :, :].rearrange("p h q -> p (h q)"),
                           False, True, [KC.b, QC.b], [pst.b])
                        pT = pT_r.next()
                        ACT(pT.t[:], pst.t[:], AF.Exp, [pst.b], [pT.b], scale=0.125)
                        for r in range(4):
                            MM(OC.t[:, r * 65:(r + 1) * 65], pT.t[:, r * 128:(r + 1) * 128], VC.t[:, gq * 65:(gq + 1) * 65],
                               kb == kbs[0] and r == 0, kb == i and r == 3, [pT.b, VC.b], [OC.b], skip=True)
                    ov = OC.t[:, 0:260].rearrange("p (h d) -> p h d", d=65)
                    TT("dve", rec.t[:, 0:4], ov[:, :, 64], esink.t[:, gq * 4:(gq + 1) * 4], ALU.add, [OC.b, esink.b], [rec.b])
                    RECIP(rec.t[:, 4:8], rec.t[:, 0:4], [rec.b], [rec.b])
                    for r in range(4):
                        c0 = 512 + (gq * 4 + r) * 64
                        TS("dve", oall.t[:, c0:c0 + 64], ov[:, r, 0:64], rec.t[:, 4 + r:5 + r], ALU.mult, [OC.b, rec.b], [oc_b])
                pt = PS.next()
                ptv = pt.t[:].bitcast(BF16).rearrange("p (k q) -> p k q", k=8)
                for kc in range(8):
                    TR(ptv[:, kc, :], oall.t[:, kc * 128:(kc + 1) * 128], ident_bf.t[:], [oall.b, oc_b, ident_bf.b], [pt.b])
                oT = oT_r.next()
                CP("act", oT.t[:], ptv, [pt.b], [oT.b])
                if debug and l == 0:
                    DMA("sp", DBGo_d[i * 128:(i + 1) * 128, :], oall.t[:], [oall.b, oc_b], [db("DBGo", i)])
                mT = mT_r.next()
                for m in range(8):
                    pm = PS.next()
                    ms = slice(m * 128, (m + 1) * 128)
                    for (br, kcs) in ((0, (0, 1)), (1, (2, 3)), (2, (4, 5, 6, 7))):
                        for kc in kcs:
                            MM(pm.t[:, br * 128:(br + 1) * 128], wbr.t[:, kc, ms], oT.t[:, kc, :], kc == 0, kc == 7,
                               [wbr.b, oT.b], [pm.b], skip=True)
                    tmp = tmp_r.next()
                    TT("dve", tmp.t[:], pm.t[:, 0:384].rearrange("p (b q) -> p b q", b=3), gT.t[:, m:24:8, :], ALU.mult, [pm.b, gT.b], [tmp.b])
                    RED(mT.t[:, m, :], tmp.t[:].rearrange("p b q -> p q b"), ALU.add, [tmp.b], [mT.b])
                r_ = r_r.next()
                for nh in range(2):
                    py = PS.next()
                    ns = slice(nh * 512, (nh + 1) * 512)
                    for m in range(8):
                        MM(py.t[:], mT.t[:, m, :], wo.t[:, m, ns], m == 0, m == 7, [mT.b, wo.b], [py.b])
                    STT(r_.t[:, ns], xblk.t[:, ns], DN_ALPHA, py.t[:], ALU.mult, ALU.add, [xblk.b, py.b], [r_.b])
                if debug and l == 0:
                    DMA("sp", DBGm_d[:, :, i * 128:(i + 1) * 128], mT.t[:], [mT.b], [db("DBGm", i)])
                    DMA("sp", DBGr_d[i * 128:(i + 1) * 128, :], r_.t[:], [r_.b], [db("DBGr", i)])
                xo = xo_r.next()
                layer_norm(r_, g1, b1, xo, lnt)
                DMA("sp", X1_d[i * 128:(i + 1) * 128, :], xo.t[:], [xo.b], [db("X1", i)])
            S.barrier()

        if "M" in stages:
            A.reset(base_mark)
            NQB = 16
            accs = [A.tile(f"macc{k}", [128, D], F32) for k in range(NQB)]
            x1T = A.tile("x1T", [128, 8, NQB * 128], BF16)
            W_r = A.ring("mw", [128, 4096], BF16, 5)
            hT_r = A.ring("hT", [128, 4, 512], BF16, 2)
            sg_r = A.ring("sg", [128, 512], BF16, 2)
            x1f_r = A.ring("x1f", [128, D], F32, 2)
            x1Tf_r = A.ring("x1Tf", [128, 8, 128], F32, 2)
            cw = A.tile("cw", [128, NQB, NE], F32)
            g2 = A.tile("g2", [128, D], F32)
            b2 = A.tile("b2", [128, D], F32)
            xo_r = A.ring("xo2", [128, D], F32, 2)
            lnt = {"st": A.tile("st2", [128, 2, 6], F32), "mv": A.tile("mv2", [128, 2], F32), "sd": A.tile("sd2", [128, 4], F32)}
            rt = {k: A.tile(f"rt_{k}", [128, NE], F32) for k in ("aff", "sel", "selm", "sel2", "e1", "e2")}
            rs = A.tile("rsm", [128, 16], F32)
            DMA("sp", g2.t[:], lnp_d[l, 2], [], [g2.b])
            DMA("sp", b2.t[:], lnp_d[l, 3], [], [b2.b])
            for qt in range(S_LEN // (NQB * 128)):
                for k in range(NQB):
                    blk = qt * NQB + k
                    x1f = x1f_r.next()
                    DMA("sp", x1f.t[:], X1_d[blk * 128:(blk + 1) * 128, :], [db("X1", blk)], [x1f.b])
                    x1Tf = x1Tf_r.next()
                    for half in range(2):
                        pt = PS.next()
                        for c in range(4):
                            kc = half * 4 + c
                            TR(pt.t[:, c * 128:(c + 1) * 128], x1f.t[:, kc * 128:(kc + 1) * 128], ident_f.t[:], [x1f.b, ident_f.b], [pt.b])
                        CP("act", x1Tf.t[:, half * 4:(half + 1) * 4, :], pt.t[:].rearrange("p (c q) -> p c q", c=4), [pt.b], [x1Tf.b])
                        CP("dve", x1T.t[:, half * 4:(half + 1) * 4, k * 128:(k + 1) * 128], pt.t[:].rearrange("p (c q) -> p c q", c=4), [pt.b], [x1T.b])
                    ACT(accs[k].t[:], x1f.t[:], AF.Copy, [x1f.b], [accs[k].b], scale=DN_ALPHA)
                    pr = PS.next()
                    for kc in range(8):
                        MM(pr.t[:, 0:NE], x1Tf.t[:, kc, :], rw.t[:, kc, :], kc == 0, kc == 7, [x1Tf.b, rw.b], [pr.b])
                    aff, sel, selm, sel2, e1, e2 = (rt[n] for n in ("aff", "sel", "selm", "sel2", "e1", "e2"))
                    ACT(aff.t[:], pr.t[:, 0:NE], AF.Sigmoid, [pr.b], [aff.b])
                    TT("dve", sel.t[:], aff.t[:], rb.t[:], ALU.add, [aff.b, rb.b], [sel.b])
                    sel3 = sel.t[:].rearrange("p (g e) -> p g e", g=4)
                    RED(rs.t[:, 0:4], sel3, ALU.max, [sel.b], [rs.b])
                    for gq in range(4):
                        TS("dve", e1.t[:, gq * 4:(gq + 1) * 4], sel.t[:, gq * 4:(gq + 1) * 4], rs.t[:, gq:gq + 1], ALU.is_equal,
                           [sel.b, rs.b], [e1.b])
                    STT(sel2.t[:], e1.t[:], -BIG, sel.t[:], ALU.mult, ALU.add, [e1.b, sel.b], [sel2.b])
                    RED(rs.t[:, 4:8], sel2.t[:].rearrange("p (g e) -> p g e", g=4), ALU.max, [sel2.b], [rs.b])
                    TT("dve", rs.t[:, 8:12], rs.t[:, 0:4], rs.t[:, 4:8], ALU.add, [rs.b], [rs.b])
                    RED(rs.t[:, 12:13], rs.t[:, 8:12], ALU.max, [rs.b], [rs.b])
                    TS("dve", rs.t[:, 8:12], rs.t[:, 8:12], rs.t[:, 12:13], ALU.is_equal, [rs.b], [rs.b])
                    TS("dve", rs.t[:, 8:12], rs.t[:, 8:12], -1.0, ALU.add, [rs.b], [rs.b], s2=BIG, op1=ALU.mult)
                    for gq in range(4):
                        TS("dve", selm.t[:, gq * 4:(gq + 1) * 4], sel.t[:, gq * 4:(gq + 1) * 4], rs.t[:, 8 + gq:9 + gq], ALU.add,
                           [sel.b, rs.b], [selm.b])
                    RED(rs.t[:, 13:14], selm.t[:], ALU.max, [selm.b], [rs.b])
                    TS("dve", e1.t[:], selm.t[:], rs.t[:, 13:14], ALU.is_equal, [selm.b, rs.b], [e1.b])
                    STT(sel2.t[:], e1.t[:], -BIG, selm.t[:], ALU.mult, ALU.add, [e1.b, selm.b], [sel2.b])
                    RED(rs.t[:, 14:15], sel2.t[:], ALU.max, [sel2.b], [rs.b])
                    TS("dve", e2.t[:], sel2.t[:], rs.t[:, 14:15], ALU.is_equal, [sel2.b, rs.b], [e2.b])
                    TT("dve", e1.t[:], e1.t[:], e2.t[:], ALU.add, [e1.b, e2.b], [e1.b])
                    TT("dve", e1.t[:], e1.t[:], aff.t[:], ALU.mult, [e1.b, aff.b], [e1.b])
                    RED(rs.t[:, 15:16], e1.t[:], ALU.add, [e1.b], [rs.b])
                    RECIP(rs.t[:, 15:16], rs.t[:, 15:16], [rs.b], [rs.b])
                    TS("dve", cw.t[:, k, :], e1.t[:], rs.t[:, 15:16], ALU.mult, [e1.b, rs.b], [cw.b])
                for e in range(NE):
                    wg = W_r.next()
                    wu = W_r.next()
                    wd = W_r.next()
                    DMA("pool", wg.t[:].rearrange("p (k n) -> p k n", k=8), wg_d[l, e].rearrange("(kc p) n -> p kc n", p=128), [], [wg.b])
                    DMA("pool", wu.t[:].rearrange("p (k n) -> p k n", k=8), wu_d[l, e].rearrange("(kc p) n -> p kc n", p=128), [], [wu.b])
                    DMA("pool", wd.t[:].rearrange("p (k n) -> p k n", k=4), wd_d[l, e].rearrange("(kc p) n -> p kc n", p=128), [], [wd.b])
                    wgv = wg.t[:].rearrange("p (k n) -> p k n", k=8)
                    wuv = wu.t[:].rearrange("p (k n) -> p k n", k=8)
                    wdv = wd.t[:].rearrange("p (k n) -> p k n", k=4)
                    for tg in range(NQB // 4):
                        tsl = slice(tg * 512, (tg + 1) * 512)
                        hT = hT_r.next()
                        for mc in range(4):
                            msl = slice(mc * 128, (mc + 1) * 128)
                            pg = PS.next()
                            pu = PS.next()
                            for kc in range(8):
                                MM(pg.t[:], wgv[:, kc, msl], x1T.t[:, kc, tsl], kc == 0, kc == 7, [wg.b, x1T.b], [pg.b])
                            for kc in range(8):
                                MM(pu.t[:], wuv[:, kc, msl], x1T.t[:, kc, tsl], kc == 0, kc == 7, [wu.b, x1T.b], [pu.b])
                            sg = sg_r.next()
                            ACT(sg.t[:], pg.t[:], AF.Silu, [pg.b], [sg.b])
                            TT("dve", hT.t[:, mc, :], sg.t[:], pu.t[:], ALU.mult, [sg.b, pu.b], [hT.b])
                        for tb in range(4):
                            k = tg * 4 + tb
                            for nh in range(2):
                                ns = slice(nh * 512, (nh + 1) * 512)
                                py = PS.next()
                                for mc in range(4):
                                    MM(py.t[:], hT.t[:, mc, tb * 128:(tb + 1) * 128], wdv[:, mc, ns], mc == 0, mc == 3, [hT.b, wd.b], [py.b])
                                STT(accs[k].t[:, ns], py.t[:], cw.t[:, k, e:e + 1], accs[k].t[:, ns], ALU.mult, ALU.add,
                                    [py.b, cw.b, accs[k].b], [accs[k].b])
                for k in range(NQB):
                    blk = qt * NQB + k
                    xo = xo_r.next()
                    layer_norm(accs[k], g2, b2, xo, lnt)
                    if l == depth - 1:
                        out_ops.append(DMA("sp", out_d[blk * 128:(blk + 1) * 128, :], xo.t[:], [xo.b], [db("out", blk)]))
                    else:
                        DMA("sp", XL_d[blk * 128:(blk + 1) * 128, :], xo.t[:], [xo.b], [db("XL", blk)])
            S.barrier()

    if not out_ops:
        out_ops = list(S.dma_last.values())
    S.emit(out_ops)
    global _LAST_S
    _LAST_S = S
    return nc


def _bf(a):
    return np.asarray(a, dtype=np.float32).astype(ml_dtypes.bfloat16)


def _consts():
    c = {}
    eye = np.eye(128, dtype=np.float32)
    c["c_ident_bf"] = _bf(eye)
    c["c_ident_f"] = eye
    c["c_i4"] = _bf(np.tile(eye, (1, 4)))
    key = np.arange(128)[:, None]
    q = np.arange(128)[None, :]
    tri = np.where(key > q, NEG, 0.0).astype(np.float32)
    band = np.where(key <= q, NEG, 0.0).astype(np.float32)
    c["c_tri4"] = _bf(np.tile(tri, (1, 4)))
    c["c_band4"] = _bf(np.tile(band, (1, 4)))
    c["c_trineg"] = np.where(q > key, -BIG, 0.0).astype(np.float32)
    E = np.zeros((32, 4096), np.float32)
    for n in range(32):
        E[n, n * 128:(n + 1) * 128] = 1.0
    c["c_E"] = _bf(E)
    c["c_pow"] = np.tile((0.5 ** np.arange(1, NBIS + 1, dtype=np.float64)).astype(np.float32)[None, :], (128, 1))
    inv = 1.0 / (10000.0 ** (np.arange(0, 64, 2, dtype=np.float32) / np.float32(64)))
    ang = np.arange(S_LEN, dtype=np.float32)[:, None] * inv[None, :].astype(np.float32)
    cos = np.cos(ang).astype(np.float32)
    sin = np.sin(ang).astype(np.float32)
    p = np.arange(128)
    cosT = cos[:, p % 32].T
    sgn = np.where((p % 64) < 32, -1.0, 1.0).astype(np.float32)
    sinT = (sin[:, p % 32].T) * sgn[:, None]
    c["cosT"] = np.ascontiguousarray(cosT, dtype=np.float32)
    c["sinT"] = np.ascontiguousarray(sinT, dtype=np.float32)
    return c


def _col_plan():
    offs = np.concatenate([[0], np.cumsum(IN_SIZES)])
    o_aq, o_ac, o_iq, o_ik, o_iw, o_bq, o_bk, o_bv, o_cq, o_ck, o_cv, o_g = offs[:12]

    def heads(c0, h0, nh):
        main, swp = [], []
        for h in range(h0, h0 + nh):
            for d in range(64):
                main.append(c0 + h * 64 + d)
                swp.append(c0 + h * 64 + (d + 32) % 64)
        return main, swp

    units = []
    units += [heads(o_aq, 0, 2), heads(o_aq, 2, 2)]
    units += [heads(o_iq, 0, 2), heads(o_iq, 2, 2)]
    m, s = heads(o_ik, 0, 1)
    units += [(m + m, s + s)]
    units += [heads(o_bq, 0, 2), heads(o_bq, 2, 2)]
    units += [heads(o_bk, 0, 2), heads(o_bk, 2, 2)]
    units += [heads(o_cq, 2 * c, 2) for c in range(4)]
    units += [heads(o_ck, 0, 2)]
    chunks = []
    for (m, s) in units:
        chunks.append(np.array(m))
        chunks.append(np.array(s))
    chunks.append(np.arange(o_ac, o_ac + 128))
    for j in range(24):
        chunks.append(np.arange(o_g + 128 * j, o_g + 128 * (j + 1)))
    assert len(chunks) == NFM
    tm = np.concatenate([np.arange(o_bv, o_bv + 256), np.arange(o_cv, o_cv + 128), np.arange(o_iw, o_iw + 4)])
    return chunks, tm


def _prep_shared(inp):
    f = lambda a: np.ascontiguousarray(np.asarray(a, dtype=np.float32))
    w_in = f(inp["w_in"])
    chunks, tm = _col_plan()
    w_fm = np.empty((DEPTH, NFM, 128, 8, 128), np.float32)
    w_tm = np.empty((DEPTH, 128, 8, NTM), np.float32)
    for l in range(DEPTH):
        wl = w_in[l].reshape(8, 128, N_IN)
        for ci, idx in enumerate(chunks):
            w_fm[l, ci] = wl[:, :, idx].transpose(1, 0, 2)
        w_tm[l] = wl[:, :, tm].transpose(1, 0, 2)
    sw = np.array([(d + 32) % 64 for d in range(64)])
    uk = f(inp["a_w_uk"])
    sh = {
        "w_fm": w_fm, "w_tm": w_tm, "uk": uk, "uksw": np.ascontiguousarray(uk[:, :, sw]), "uv": f(inp["a_w_uv"]),
        "w_branch": f(inp["w_branch"]), "w_o": f(inp["w_o"]),
        "moe_w_gate": f(inp["moe_w_gate"]), "moe_w_up": f(inp["moe_w_up"]), "moe_w_down": f(inp["moe_w_down"]),
    }
    lnp = np.empty((DEPTH, 4, 128, D), np.float32)
    for l in range(DEPTH):
        for k, nm in enumerate(("ln1_g", "ln1_b", "ln2_g", "ln2_b")):
            lnp[l, k] = np.broadcast_to(f(inp[nm])[l][None, :], (128, D))
    sh["lnp"] = lnp
    sh["sinks_bc"] = np.ascontiguousarray(np.broadcast_to(f(inp["c_sinks"])[:, None, :], (DEPTH, 128, 8)))
    sh["rw"] = np.ascontiguousarray(f(inp["router_w"]).reshape(8, 128, NE).transpose(1, 0, 2))
    sh["rb_bc"] = np.ascontiguousarray(np.broadcast_to(f(inp["router_b"])[None, :], (128, NE)))
    sh.update(_consts())
    return sh


_NC_CACHE = {}


def kernel(**inputs):
    x = np.ascontiguousarray(np.asarray(inputs["x"], dtype=np.float32))
    sh = _prep_shared(inputs)
    if "nc" not in _NC_CACHE:
        _NC_CACHE["nc"] = build_program()
    nc = _NC_CACHE["nc"]
    in_maps = []
    for b in range(NCORES):
        m = dict(sh)
        m["x"] = x[b]
        in_maps.append(m)
    res = run_bass_kernel_spmd(nc, in_maps, core_ids=list(range(NCORES)))
    out = np.stack([np.asarray(res.results[b]["out"], dtype=np.float32) for b in range(NCORES)], axis=0)
    return out
```

```python
import bisect
import math
import numpy as np
import ml_dtypes
import concourse.bass as bass
import concourse.mybir as mybir
from concourse.bass_utils import run_bass_kernel_spmd

F32 = mybir.dt.float32
BF16 = mybir.dt.bfloat16
AF = mybir.ActivationFunctionType
ALU = mybir.AluOpType
AX = mybir.AxisListType

NCORES = 4
S_LEN = 8192
D = 1024
NB = S_LEN // 128
NG = S_LEN // 512
DEPTH = 2
IN_SIZES = (256, 128, 256, 64, 4, 256, 256, 256, 512, 128, 128, 3072)
N_IN = sum(IN_SIZES)
NFM = 53
NTM = 388
DN_ALPHA = (2 * DEPTH) ** 0.25
LN_EPS = 1e-5
NEG = -30000.0
BIG = 1.0e30
TOPK = 256
NBIS = 14
NE = 16
DE = 512


class Op:
    __slots__ = ("eng", "fn", "waits", "sig", "idx", "dma_sem")

    def __init__(self, eng, fn):
        self.eng = eng
        self.fn = fn
        self.waits = []
        self.sig = None
        self.idx = None
        self.dma_sem = None


class Buf:
    __slots__ = ("name", "lw", "rd", "excl")

    def __init__(self, name="", excl=False):
        self.name = name
        self.lw = None
        self.rd = []
        self.excl = excl


class T:
    __slots__ = ("t", "b")

    def __init__(self, t, b):
        self.t = t
        self.b = b


class Ring:
    def __init__(self, items):
        self.items = items
        self.i = 0

    def next(self):
        it = self.items[self.i % len(self.items)]
        self.i += 1
        return it


class Sched:
    ENGS = ("pe", "act", "dve", "pool", "sp")

    def __init__(self, nc, n_dma_sems=16):
        self.nc = nc
        self.ops = {e: [] for e in self.ENGS}
        self.count = {e: 0 for e in self.ENGS}
        self.sig_idx = {e: [] for e in self.ENGS}
        self.known = {e: {} for e in self.ENGS}
        self.n_dma_sems = n_dma_sems
        self.dma_cnt = {}
        self.dma_rr = {e: 0 for e in self.ENGS}
        self.dma_last = {}
        self.last_real = {e: None for e in self.ENGS}

    def ticket(self, op):
        if op.dma_sem is not None:
            return op.dma_sem
        e = op.eng
        if op.sig is not None:
            return (e, op.sig)
        lst = self.sig_idx[e]
        j = bisect.bisect_left(lst, op.idx)
        if j < len(lst):
            return (e, self.ops[e][lst[j]].sig)
        self.count[e] += 1
        op.sig = self.count[e]
        lst.append(op.idx)
        return (e, op.sig)

    def need(self, op, dep):
        if dep is None:
            return
        if dep.eng == "pe" and op.eng == "pe" and dep.dma_sem is None and op.dma_sem is None:
            return
        import os as _o
        if _o.environ.get("NOSELF") == "1" and dep.eng == op.eng and dep.dma_sem is None and op.dma_sem is None:
            return
        k, v = self.ticket(dep)
        kn = self.known[op.eng]
        if kn.get(k, 0) >= v:
            return
        kn[k] = v
        op.waits.append((k, v))

    def add(self, eng, fn, reads=(), writes=(), dma=False):
        op = Op(eng, fn)
        op.idx = len(self.ops[eng])
        if dma:
            op.dma_sem = ("pending", 0)
        for b in reads:
            self.need(op, b.lw)
            if b.excl:
                last = {}
                for r in b.rd:
                    if r.eng != eng and (r.eng not in last or r.idx > last[r.eng].idx):
                        last[r.eng] = r
                for r in last.values():
                    self.need(op, r)
        for b in writes:
            self.need(op, b.lw)
            last = {}
            for r in b.rd:
                if r.dma_sem is not None:
                    self.need(op, r)
                elif r.eng not in last or r.idx > last[r.eng].idx:
                    last[r.eng] = r
            for r in last.values():
                self.need(op, r)
        if dma:
            j = self.dma_rr[eng]
            self.dma_rr[eng] = (j + 1) % self.n_dma_sems
            key = ("dma", eng, j)
            prev = self.dma_last.get(key)
            if prev is not None:
                self.need(op, prev)
            self.dma_cnt[key] = self.dma_cnt.get(key, 0) + 1
            op.dma_sem = (key, 16 * self.dma_cnt[key])
            self.dma_last[key] = op
        self.ops[eng].append(op)
        self.last_real[eng] = op
        for b in writes:
            b.lw = op
            b.rd = []
        for b in reads:
            b.rd.append(op)
        return op

    def barrier(self):
        deps = [o for o in self.last_real.values() if o is not None and o.dma_sem is None]
        deps += list(self.dma_last.values())
        for e in self.ENGS:
            op = Op(e, None)
            op.idx = len(self.ops[e])
            for d in deps:
                if d.eng == e and d.dma_sem is None:
                    continue
                self.need(op, d)
            self.ops[e].append(op)

    def emit(self, final_ops):
        nc = self.nc
        fin = Op("sp", None)
        fin.idx = len(self.ops["sp"])
        for o in final_ops:
            self.need(fin, o)
        self.ops["sp"].append(fin)
        semkeys = [e for e in self.ENGS if self.count[e] > 0] + list(self.dma_cnt.keys())
        sems = {}
        for i, k in enumerate(semkeys):
            sems[k] = nc.alloc_semaphore(name=f"sem{i}")
        with nc.allow_low_precision(reason="bf16 matmul operands, fp32 accumulation"), nc.Block() as block:
            def mk(e):
                def body(h):
                    for op in self.ops[e]:
                        for (k, v) in op.waits:
                            h.wait_ge(sems[k], v)
                        if op.fn is None:
                            continue
                        inst = op.fn(h)
                        if op.dma_sem is not None:
                            inst.then_inc(sems[op.dma_sem[0]], 16)
                        elif op.sig is not None:
                            inst.then_inc(sems[e], 1)
                return body
            block.tensor(mk("pe"))
            block.scalar(mk("act"))
            block.vector(mk("dve"))
            block.gpsimd(mk("pool"))
            block.sync(mk("sp"))


class Alloc:
    def __init__(self, nc, base, top):
        self.nc = nc
        self.cur = base
        self.top = top
        self.n = 0

    def mark(self):
        return self.cur

    def reset(self, m):
        self.cur = m

    def tile(self, name, shape, dtype, nbuf=None):
        size = 2 if dtype == BF16 else 4
        nbytes = size
        for s in shape[1:]:
            nbytes *= s
        off = (self.cur + 63) // 64 * 64
        assert off + nbytes <= self.top, f"SBUF overflow allocating {name}: {off}+{nbytes} > {self.top}"
        self.n += 1
        t = self.nc.alloc_sbuf_tensor_at(f"{name}_{self.n}", list(shape), dtype, offset=off)
        self.cur = off + nbytes
        return T(t, Buf(name))

    def ring(self, name, shape, dtype, n):
        return Ring([self.tile(f"{name}{i}", shape, dtype) for i in range(n)])


def build_program(debug=False, stages=("P", "A1", "A2", "M"), depth=DEPTH):
    nc = bass.Bass("TRN2", target_bir_lowering=False)
    import os as _os
    S = Sched(nc, n_dma_sems=int(_os.environ.get("NDS", "16")))

    def din(name, shape, dt=F32):
        return nc.dram_tensor(name, list(shape), dt, kind="ExternalInput").ap()

    scr_kind = "ExternalOutput" if debug else "Internal"

    def dscr(name, shape, dt):
        return nc.dram_tensor(name, list(shape), dt, kind=scr_kind).ap()

    x_d = din("x", [S_LEN, D])
    wfm_d = din("w_fm", [DEPTH, NFM, 128, 8, 128])
    wtm_d = din("w_tm", [DEPTH, 128, 8, NTM])
    uk_d = din("uk", [DEPTH, 128, 64])
    uksw_d = din("uksw", [DEPTH, 128, 64])
    uv_d = din("uv", [DEPTH, 128, 64])
    wbr_d = din("w_branch", [DEPTH, D, D])
    wo_d = din("w_o", [DEPTH, D, D])
    lnp_d = din("lnp", [DEPTH, 4, 128, D])
    sinks_d = din("sinks_bc", [DEPTH, 128, 8])
    rw_d = din("rw", [128, 8, NE])
    rb_d = din("rb_bc", [128, NE])
    wg_d = din("moe_w_gate", [DEPTH, NE, D, DE])
    wu_d = din("moe_w_up", [DEPTH, NE, D, DE])
    wd_d = din("moe_w_down", [DEPTH, NE, DE, D])
    cos_d = din("cosT", [128, S_LEN])
    sin_d = din("sinT", [128, S_LEN])
    c_ident_bf = din("c_ident_bf", [128, 128], BF16)
    c_ident_f = din("c_ident_f", [128, 128])
    c_i4 = din("c_i4", [128, 512], BF16)
    c_tri4 = din("c_tri4", [128, 512], BF16)
    c_band4 = din("c_band4", [128, 512], BF16)
    c_trineg = din("c_trineg", [128, 128])
    c_E = din("c_E", [32, 4096], BF16)
    c_pow = din("c_pow", [128, NBIS])
    out_d = nc.dram_tensor("out", [S_LEN, D], F32, kind="ExternalOutput").ap()

    QA_d = dscr("QA_d", [64, 4, S_LEN], BF16)
    IQ_d = dscr("IQ_d", [64, 4, S_LEN], BF16)
    IK_d = dscr("IK_d", [64, S_LEN], BF16)
    KA_d = dscr("KA_d", [64, S_LEN], BF16)
    QB_d = dscr("QB_d", [128, 2, S_LEN], BF16)
    KB_d = dscr("KB_d", [128, 2, S_LEN], BF16)
    QC_d = dscr("QC_d", [128, 4, S_LEN], BF16)
    KC_d = dscr("KC_d", [128, S_LEN], BF16)
    GT_d = dscr("GT_d", [128, 24, S_LEN], BF16)
    VA_d = dscr("VA_d", [128, NB, 65], BF16)
    VB_d = dscr("VB_d", [128, NB, 260], BF16)
    VC_d = dscr("VC_d", [S_LEN, 130], BF16)
    IW_d = dscr("IW_d", [S_LEN, 8], F32)
    OAB_d = dscr("OAB_d", [S_LEN, 512], BF16)
    X1_d = dscr("X1_d", [S_LEN, D], F32)
    XL_d = dscr("XL_d", [S_LEN, D], F32)
    if debug:
        DBGo_d = dscr("DBGo_d", [S_LEN, D], BF16)
        DBGm_d = dscr("DBGm_d", [128, 8, S_LEN], BF16)
        DBGr_d = dscr("DBGr_d", [S_LEN, D], F32)
    DB = {}

    def db(name, i):
        k = (name, i)
        if k not in DB:
            DB[k] = Buf(f"{name}{i}")
        return DB[k]

    def dbg_all(name):
        return [db(name, g) for g in range(NG)]

    def MM(out, lhsT, rhs, start, stop, R, W, skip=False):
        S.add("pe", lambda h: h.matmul(out, lhsT=lhsT, rhs=rhs, start=start, stop=stop, skip_group_check=skip), reads=R, writes=W)

    def TR(out, in_, ident, R, W):
        S.add("pe", lambda h: h.transpose(out=out, in_=in_, identity=ident), reads=R, writes=W)

    def ACT(out, in_, func, R, W, scale=1.0, bias=None, accum=None):
        if bias is None:
            S.add("act", lambda h: h.activation(out=out, in_=in_, func=func, scale=scale), reads=R, writes=W)
        else:
            S.add("act", lambda h: h.activation(out=out, in_=in_, func=func, scale=scale, bias=bias), reads=R, writes=W)

    def TS(eng, out, in0, s1, op0, R, W, s2=None, op1=None, accum=None):
        def f(h):
            kw = {}
            if op1 is not None:
                kw["op1"] = op1
            if accum is not None:
                kw["accum_out"] = accum
            return h.tensor_scalar(out=out, in0=in0, scalar1=s1, scalar2=s2, op0=op0, **kw)
        S.add(eng, f, reads=R, writes=W)

    def TT(eng, out, in0, in1, op, R, W):
        S.add(eng, lambda h: h.tensor_tensor(out=out, in0=in0, in1=in1, op=op), reads=R, writes=W)

    def STT(out, in0, scalar, in1, op0, op1, R, W):
        S.add("dve", lambda h: h.scalar_tensor_tensor(out=out, in0=in0, scalar=scalar, in1=in1, op0=op0, op1=op1), reads=R, writes=W)

    def RED(out, in_, op, R, W, axis=AX.X):
        S.add("dve", lambda h: h.tensor_reduce(out=out, in_=in_, axis=axis, op=op), reads=R, writes=W)

    def CP(eng, out, in_, R, W):
        if eng == "act":
            S.add("act", lambda h: h.activation(out=out, in_=in_, func=AF.Copy), reads=R, writes=W)
        else:
            S.add(eng, lambda h: h.tensor_copy(out=out, in_=in_), reads=R, writes=W)

    def RECIP(out, in_, R, W):
        S.add("dve", lambda h: h.reciprocal(out=out, in_=in_), reads=R, writes=W)

    def MAX8(out, in_, R, W):
        S.add("dve", lambda h: h.max(out=out, in_=in_), reads=R, writes=W)

    def MSET(eng, ap, val, W):
        S.add(eng, lambda h: h.memset(ap, val), writes=W)

    def DMA(eng, out, in_, R, W, mdl=4096):
        if eng == "pool":
            return S.add(eng, lambda h: h.dma_start(out=out, in_=in_, max_dma_last_dim=mdl), reads=R, writes=W, dma=True)
        return S.add(eng, lambda h: h.dma_start(out=out, in_=in_), reads=R, writes=W, dma=True)

    A = Alloc(nc, 16512, 229344)
    PSB = [T(nc.alloc_psum_tensor(f"psb{i}", [128, 512], F32), Buf(f"psb{i}", excl=True)) for i in range(8)]
    PS = Ring(PSB[0:6])
    PO = Ring(PSB[6:8])

    ident_bf = A.tile("ident_bf", [128, 128], BF16)
    ident_f = A.tile("ident_f", [128, 128], F32)
    i4 = A.tile("i4", [128, 512], BF16)
    tri4 = A.tile("tri4", [128, 512], BF16)
    band4 = A.tile("band4", [128, 512], BF16)
    trineg = A.tile("trineg", [128, 128], F32)
    Ec = A.tile("Ec", [32, 4096], BF16)
    cpow = A.tile("cpow", [128, NBIS], F32)
    neg1 = A.tile("neg1", [128, 1], F32)
    rw = A.tile("rw", [128, 8, NE], F32)
    rb = A.tile("rb", [128, NE], F32)
    for tl, src in ((ident_bf, c_ident_bf), (ident_f, c_ident_f), (i4, c_i4), (tri4, c_tri4), (band4, c_band4),
                    (trineg, c_trineg), (Ec, c_E), (cpow, c_pow), (rw, rw_d), (rb, rb_d)):
        DMA("sp", tl.t[:], src, [], [tl.b])
    MSET("dve", neg1.t[:], -1.0, [neg1.b])
    base_mark = A.mark()

    out_ops = []

    def layer_norm(r, g_bc, b_bc, xo, tmp):
        st, mv, sd = tmp["st"], tmp["mv"], tmp["sd"]
        S.add("dve", lambda h: h.bn_stats(out=st.t[:, 0, :], in_=r.t[:, 0:512]), reads=[r.b], writes=[st.b])
        S.add("dve", lambda h: h.bn_stats(out=st.t[:, 1, :], in_=r.t[:, 512:1024]), reads=[r.b], writes=[st.b])
        S.add("dve", lambda h: h.bn_aggr(out=mv.t[:], in_=st.t[:]), reads=[st.b], writes=[mv.b])
        TS("dve", sd.t[:, 0:1], mv.t[:, 1:2], LN_EPS, ALU.add, [mv.b], [sd.b])
        ACT(sd.t[:, 1:2], sd.t[:, 0:1], AF.Sqrt, [sd.b], [sd.b])
        S.add("dve", lambda h: h.reciprocal(out=sd.t[:, 2:3], in_=sd.t[:, 1:2]), reads=[sd.b], writes=[sd.b])
        TS("dve", r.t[:], r.t[:], mv.t[:, 0:1], ALU.subtract, [r.b, mv.b, sd.b], [r.b], s2=sd.t[:, 2:3], op1=ALU.mult)
        TT("dve", r.t[:], r.t[:], g_bc.t[:], ALU.mult, [r.b, g_bc.b], [r.b])
        TT("dve", xo.t[:], r.t[:], b_bc.t[:], ALU.add, [r.b, b_bc.b], [xo.b])

    for l in range(depth):
        xin_d = x_d if l == 0 else XL_d
        xin_name = "x" if l == 0 else "XL"

        if "P" in stages:
            A.reset(base_mark)
            Wfm = [A.tile(f"wfm{ci}", [128, 8, 128], BF16) for ci in range(NFM)]
            Wtm = A.tile("wtm", [128, 8, NTM], BF16)
            ukt = A.tile("uk", [128, 64], BF16)
            ukswt = A.tile("uksw", [128, 64], BF16)
            uvt = A.tile("uv", [128, 64], BF16)
            xf_r = A.ring("xf", [128, D], F32, 8)
            xb_r = A.ring("xb", [128, D], BF16, 2)
            xT_r = A.ring("xT", [128, 8, 512], BF16, 2)
            cs_r = A.ring("cs", [128, 512], F32, 2)
            sn_r = A.ring("sn", [128, 512], F32, 2)
            t1_r = A.ring("t1", [128, 512], F32, 2)
            t2_r = A.ring("t2", [128, 512], F32, 2)
            ost_r = A.ring("ost", [128, 512], BF16, 4)
            acT_r = A.ring("acT", [128, 512], BF16, 2)
            vast_r = A.ring("vast", [128, 65], BF16, 2)
            vbst_r = A.ring("vbst", [128, 4, 65], BF16, 2)
            vcst_r = A.ring("vcst", [128, 2, 65], BF16, 2)
            iwst_r = A.ring("iwst", [128, 8], F32, 2)
            for rg in (vast_r, vbst_r, vcst_r):
                for tl in rg.items:
                    MSET("dve", tl.t[:], 1.0, [tl.b])
            order = list(range(NFM))
            for ci in order:
                DMA("pool", Wfm[ci].t[:], wfm_d[l, ci], [], [Wfm[ci].b])
            DMA("pool", Wtm.t[:], wtm_d[l], [], [Wtm.b], mdl=NTM * 4)
            DMA("pool", ukt.t[:], uk_d[l], [], [ukt.b])
            DMA("pool", ukswt.t[:], uksw_d[l], [], [ukswt.b])
            DMA("pool", uvt.t[:], uv_d[l], [], [uvt.b])

            def load_group(g):
                res = []
                for tb in range(4):
                    blk = g * 4 + tb
                    xf = xf_r.next()
                    DMA("sp", xf.t[:], xin_d[blk * 128:(blk + 1) * 128, :], [db(xin_name, blk)], [xf.b])
                    res.append(xf)
                cs = cs_r.next()
                sn = sn_r.next()
                DMA("sp", cs.t[:], cos_d[:, g * 512:(g + 1) * 512], [], [cs.b])
                DMA("sp", sn.t[:], sin_d[:, g * 512:(g + 1) * 512], [], [sn.b])
                return res, cs, sn

            def rope_out(pa, pb_, cs, sn, np_):
                t1 = t1_r.next()
                t2 = t2_r.next()
                ost = ost_r.next()
                TT("dve", t1.t[0:np_, :], pa.t[0:np_, :], cs.t[0:np_, :], ALU.mult, [pa.b, cs.b], [t1.b])
                TT("dve", t2.t[0:np_, :], pb_.t[0:np_, :], sn.t[0:np_, :], ALU.mult, [pb_.b, sn.b], [t2.b])
                TT("dve", ost.t[0:np_, :], t1.t[0:np_, :], t2.t[0:np_, :], ALU.add, [t1.b, t2.b], [ost.b])
                return ost

            def unit_dst(u, ts):
                if u in (0, 1):
                    return [(QA_d[:, 2 * u, ts], 0, 64), (QA_d[:, 2 * u + 1, ts], 64, 128)], "QA"
                if u in (2, 3):
                    c = u - 2
                    return [(IQ_d[:, 2 * c, ts], 0, 64), (IQ_d[:, 2 * c + 1, ts], 64, 128)], "IQ"
                if u == 4:
                    return [(IK_d[:, ts], 0, 64)], "IK"
                if u in (5, 6):
                    return [(QB_d[:, u - 5, ts], 0, 128)], "QB"
                if u in (7, 8):
                    return [(KB_d[:, u - 7, ts], 0, 128)], "KB"
                if u in (9, 10, 11, 12):
                    c = u - 9
                    res = []
                    for half in range(2):
                        hh = 2 * c + half
                        gq, r = hh // 4, hh % 4
                        res.append((QC_d[gq * 64:(gq + 1) * 64, r, ts], half * 64, half * 64 + 64))
                    return res, "QC"
                return [(KC_d[:, ts], 0, 128)], "KC"

            import os
            PD = int(os.environ.get("PDBG", "15"))
            NGR = int(os.environ.get("PNG", str(NG)))
            nxt = load_group(0)
            for g in range(NGR):
                xfs, cs, sn = nxt
                if g + 1 < NGR:
                    nxt = load_group(g + 1)
                ts = slice(g * 512, (g + 1) * 512)
                xT = xT_r.next()
                for tb in range(4):
                    xb = xb_r.next()
                    CP("dve" if tb % 2 == 0 else "act", xb.t[:], xfs[tb].t[:], [xfs[tb].b], [xb.b])
                    pt = PS.next()
                    ptv = pt.t[:].bitcast(BF16).rearrange("p (k q) -> p k q", k=8)
                    for kc in range(8):
                        TR(ptv[:, kc, :], xb.t[:, kc * 128:(kc + 1) * 128], ident_bf.t[:], [xb.b, ident_bf.b], [pt.b])
                    CP("act" if tb % 2 == 0 else "dve", xT.t[:, :, tb * 128:(tb + 1) * 128], ptv, [pt.b], [xT.b])
                ulist = [int(v) for v in os.environ.get("PUNITS", ",".join(str(v) for v in range(14))).split(",")]
                for u in (ulist if PD & 1 else []):
                    pa = PS.next()
                    pb_ = PS.next()
                    for kc in range(8):
                        MM(pa.t[:], Wfm[2 * u].t[:, kc, :], xT.t[:, kc, :], kc == 0, kc == 7, [Wfm[2 * u].b, xT.b], [pa.b])
                    for kc in range(8):
                        MM(pb_.t[:], Wfm[2 * u + 1].t[:, kc, :], xT.t[:, kc, :], kc == 0, kc == 7, [Wfm[2 * u + 1].b, xT.b], [pb_.b])
                    dsts, nm = unit_dst(u, ts)
                    np_ = 64 if u == 4 else 128
                    ost = rope_out(pa, pb_, cs, sn, np_)
                    for (dap, p0, p1) in dsts:
                        DMA("sp", dap, ost.t[p0:p1, :], [ost.b], [db(nm, g)])
                if not (PD & 2):
                    continue
                pa = PS.next()
                for kc in range(8):
                    MM(pa.t[:], Wfm[28].t[:, kc, :], xT.t[:, kc, :], kc == 0, kc == 7, [Wfm[28].b, xT.b], [pa.b])
                acT = acT_r.next()
                CP("act", acT.t[:], pa.t[:], [pa.b], [acT.b])
                pa = PS.next()
                pb_ = PS.next()
                MM(pa.t[0:64, :], ukt.t[:], acT.t[:], True, True, [ukt.b, acT.b], [pa.b])
                MM(pb_.t[0:64, :], ukswt.t[:], acT.t[:], True, True, [ukswt.b, acT.b], [pb_.b])
                ost = rope_out(pa, pb_, cs, sn, 64)
                DMA("sp", KA_d[:, ts], ost.t[0:64, :], [ost.b], [db("KA", g)])
                for tb in range(4):
                    blk = g * 4 + tb
                    pv = PS.next()
                    MM(pv.t[:, 0:64], acT.t[:, tb * 128:(tb + 1) * 128], uvt.t[:], True, True, [acT.b, uvt.b], [pv.b])
                    vast = vast_r.next()
                    CP("act", vast.t[:, 0:64], pv.t[:, 0:64], [pv.b], [vast.b])
                    DMA("sp", VA_d[:, blk, :], vast.t[:], [vast.b], [db("VA", g)])
                for j in range(24 if PD & 4 else 0):
                    pa = PS.next()
                    W_ = Wfm[29 + j]
                    for kc in range(8):
                        MM(pa.t[:], W_.t[:, kc, :], xT.t[:, kc, :], kc == 0, kc == 7, [W_.b, xT.b], [pa.b])
                    ost = ost_r.next()
                    ACT(ost.t[:], pa.t[:], AF.Sigmoid, [pa.b], [ost.b])
                    DMA("sp", GT_d[:, j, ts], ost.t[:], [ost.b], [db("GT", g)])
                for tb in range(4 if PD & 8 else 0):
                    blk = g * 4 + tb
                    pv = PS.next()
                    for kc in range(8):
                        MM(pv.t[:, 0:NTM], xT.t[:, kc, tb * 128:(tb + 1) * 128], Wtm.t[:, kc, :], kc == 0, kc == 7, [xT.b, Wtm.b], [pv.b])
                    vbst = vbst_r.next()
                    vcst = vcst_r.next()
                    iwst = iwst_r.next()
                    TMO = int(os.environ.get("TMO", "7"))
                    if TMO & 1:
                        CP("act", vbst.t[:, :, 0:64], pv.t[:, 0:256].rearrange("p (h d) -> p h d", h=4), [pv.b], [vbst.b])
                    if TMO & 2:
                        CP("dve", vcst.t[:, :, 0:64], pv.t[:, 256:384].rearrange("p (h d) -> p h d", h=2), [pv.b], [vcst.b])
                    if TMO & 4:
                        CP("dve", iwst.t[:, 4:8], pv.t[:, 384:388], [pv.b], [iwst.b])
                        STT(iwst.t[:, 0:4], pv.t[:, 384:388], -1.0, iwst.t[:, 4:8], ALU.mult, ALU.max, [pv.b, iwst.b], [iwst.b])
                    TMD = int(os.environ.get("TMD", "15"))
                    if TMD & 1:
                        ACT(iwst.t[:, 4:8], pv.t[:, 384:388], AF.Sign, [pv.b], [iwst.b])
                    if TMD & 2:
                        DMA("sp", VB_d[:, blk, :], vbst.t[:].rearrange("p h d -> p (h d)"), [vbst.b], [db("VB", g)])
                    if TMD & 4:
                        DMA("sp", VC_d[blk * 128:(blk + 1) * 128, :], vcst.t[:].rearrange("p h d -> p (h d)"), [vcst.b], [db("VC", blk)])
                    if TMD & 8:
                        DMA("sp", IW_d[blk * 128:(blk + 1) * 128, :], iwst.t[:], [iwst.b], [db("IW", blk)])
            S.barrier()

        if "A1" in stages:
            A.reset(base_mark)
            KI = A.tile("KI", [128, S_LEN], BF16)
            ka_b, ik_b = Buf("ka"), Buf("ik")
            VA = A.tile("VA", [128, NB, 65], BF16)
            KBt = A.tile("KB", [128, 2, S_LEN], BF16)
            VBt = A.tile("VB", [128, NB, 260], BF16)
            acc = A.tile("acc", [128, S_LEN], F32)
            mb = A.tile("mb", [128, S_LEN], BF16)
            kms = A.tile("kms", [128, 2, 32], F32)
            kmT = A.tile("kmT", [128, 2, 32], BF16)
            gs = A.tile("gs", [128, 4, 32], F32)
            m8 = A.tile("m8", [128, 4, 8], F32)
            mbm = A.tile("mbm", [128, 4, 32], BF16)
            mbT = A.tile("mbT", [32, 512], BF16)
            QI_r = A.ring("QI", [128, 4, 128], BF16, 2)
            QA_r = A.ring("QAz", [128, 4, 128], BF16, 2)
            QB_r = A.ring("QB", [128, 4, 128], BF16, 2)
            for rg in (QI_r, QA_r, QB_r):
                for tl in rg.items:
                    MSET("dve", tl.t[:], 0.0, [tl.b])
            IW_r = A.ring("IW", [128, 8], F32, 2)
            pT_r = A.ring("pT", [128, 512], BF16, 4)
            oab_r = A.ring("oab", [128, 512], BF16, 2)
            bs = A.tile("bs", [128, 8], F32)
            HW = A.tile("HW", [128, NBIS], F32)
            rec = A.tile("rec", [128, 8], F32)

            DMA("sp", KI.t[0:64, :], KA_d, dbg_all("KA"), [ka_b])
            DMA("sp", KI.t[64:128, :], IK_d, dbg_all("IK"), [ik_b])
            DMA("sp", VA.t[:], VA_d, dbg_all("VA"), [VA.b])
            for c in range(2):
                DMA("sp", KBt.t[:, c, :], KB_d[:, c, :], dbg_all("KB"), [KBt.b])
            for c in range(4):
                DMA("sp", VBt.t[:, c * 16:(c + 1) * 16, :], VB_d[:, c * 16:(c + 1) * 16, :], dbg_all("VB"), [VBt.b])
            MSET("dve", gs.t[:], -BIG, [gs.b])
            for c in range(2):
                RED(kms.t[:, c, :], KBt.t[:, c, :].rearrange("p (n k) -> p n k", k=256), ALU.add, [KBt.b], [kms.b])
            TS("dve", kmT.t[:], kms.t[:], 1.0 / 256.0, ALU.mult, [kms.b], [kmT.b])

            def load_a1(i):
                QI = QI_r.next()
                QAz = QA_r.next()
                QB = QB_r.next()
                IW = IW_r.next()
                bsl = slice(i * 128, (i + 1) * 128)
                g = i // 4
                DMA("sp", QAz.t[0:64, :, :], QA_d[:, :, bsl], [db("QA", g)], [QAz.b])
                DMA("sp", QI.t[64:128, :, :], IQ_d[:, :, bsl], [db("IQ", g)], [QI.b])
                for h in range(4):
                    p0 = (h % 2) * 64
                    DMA("sp", QB.t[p0:p0 + 64, h, :], QB_d[p0:p0 + 64, h // 2, bsl], [db("QB", g)], [QB.b])
                DMA("sp", IW.t[:], IW_d[bsl, :], [db("IW", i)], [IW.b])
                return QI, QAz, QB, IW

            import os
            NB1 = int(os.environ.get("A1NB", str(NB)))
            AP_ = int(os.environ.get("A1PARTS", "31"))
            nxt = load_a1(0)
            for i in range(NB1):
                QI, QAz, QB, IW = nxt
                if i + 1 < NB1:
                    nxt = load_a1(i + 1)
                nk = 128 * (i + 1)
                cur = i // 2
                if cur > 0 and (AP_ & 1):
                    pg = PS.next()
                    for h in range(4):
                        p0 = (h % 2) * 64
                        MM(pg.t[:, h * 32:(h + 1) * 32], QB.t[:, h, :], kmT.t[:, h // 2, :],
                           h == 0, h == 3, [QB.b, kmT.b], [pg.b], skip=True)
                    CP("dve", gs.t[:, :, 0:cur], pg.t[:, 0:128].rearrange("p (h n) -> p h n", h=4)[:, :, 0:cur], [pg.b], [gs.b])
                    for h in range(4):
                        MAX8(m8.t[:, h, :], gs.t[:, h, :], [gs.b], [m8.b])
                    for h in range(4):
                        TS("dve", mbm.t[:, h, :], gs.t[:, h, :], m8.t[:, h, 2:3], ALU.is_lt, [gs.b, m8.b], [mbm.b], s2=NEG, op1=ALU.mult)
                    pt = PS.next()
                    ptv = pt.t[:].bitcast(BF16)
                    for h in range(4):
                        TR(ptv[0:32, h * 128:(h + 1) * 128], mbm.t[:, h, :], ident_bf.t[:], [mbm.b, ident_bf.b], [pt.b])
                    CP("act", mbT.t[:], ptv[0:32, 0:512], [pt.b], [mbT.b])
                if i >= 2 and (AP_ & 2):
                    ntile = (nk + 511) // 512
                    for j in range(ntile):
                        w = min(512, nk - 512 * j)
                        ks = slice(j * 512, j * 512 + w)
                        for h in range(4):
                            pi = PS.next()
                            MM(pi.t[:, 0:w], QI.t[:, h, :], KI.t[:, ks], True, True, [QI.b, ik_b, ka_b], [pi.b])
                            ACT(pi.t[:, 0:w], pi.t[:, 0:w], AF.Relu, [pi.b, IW.b], [pi.b], scale=IW.t[:, h:h + 1])
                            if h == 0:
                                TS("dve", acc.t[:, ks], pi.t[:, 0:w], IW.t[:, 4:5], ALU.mult, [pi.b, IW.b], [acc.b])
                            else:
                                STT(acc.t[:, ks], pi.t[:, 0:w], IW.t[:, 4 + h:5 + h], acc.t[:, ks], ALU.mult, ALU.add, [pi.b, IW.b, acc.b], [acc.b])
                    dsl = slice(i * 128, (i + 1) * 128)
                    TT("dve", acc.t[:, dsl], acc.t[:, dsl], trineg.t[:], ALU.add, [acc.b, trineg.b], [acc.b])
                    RED(bs.t[:, 5:6], acc.t[:, 0:nk], ALU.max, [acc.b], [bs.b])
                    RED(bs.t[:, 0:1], acc.t[:, 0:i * 128], ALU.min, [acc.b], [bs.b])
                    TT("dve", bs.t[:, 1:2], bs.t[:, 5:6], bs.t[:, 0:1], ALU.subtract, [bs.b], [bs.b])
                    TS("dve", HW.t[:], cpow.t[:], bs.t[:, 1:2], ALU.mult, [cpow.b, bs.b], [HW.b])
                    for k in range(NBIS):
                        TT("dve", bs.t[:, 2:3], bs.t[:, 0:1], HW.t[:, k:k + 1], ALU.add, [bs.b, HW.b], [bs.b])
                        TS("dve", mb.t[:, 0:nk], acc.t[:, 0:nk], bs.t[:, 2:3], ALU.is_ge, [acc.b, bs.b], [mb.b, bs.b],
                           s2=None, op1=ALU.add, accum=bs.t[:, 3:4])
                        TS("dve", bs.t[:, 4:5], bs.t[:, 3:4], TOPK - 0.5, ALU.is_ge, [bs.b], [bs.b])
                        STT(bs.t[:, 0:1], HW.t[:, k:k + 1], bs.t[:, 4:5], bs.t[:, 0:1], ALU.mult, ALU.add, [bs.b, HW.b], [bs.b])
                    thr = bs.t[:, 0:1]
                    thr_b = bs.b
                else:
                    MSET("dve", acc.t[:, 0:nk], 0.0, [acc.b])
                    dsl = slice(i * 128, (i + 1) * 128)
                    TT("dve", acc.t[:, dsl], acc.t[:, dsl], trineg.t[:], ALU.add, [acc.b, trineg.b], [acc.b])
                    thr = neg1.t[:, 0:1]
                    thr_b = neg1.b
                TS("dve", mb.t[:, 0:nk], acc.t[:, 0:nk], thr, ALU.is_lt, [acc.b, thr_b], [mb.b], s2=NEG, op1=ALU.mult)
                OB = PO.next()
                for kb in range(i + 1 if AP_ & 4 else 0):
                    ks = slice(kb * 128, (kb + 1) * 128)
                    pst = PS.next()
                    first = True
                    if kb < 2 * cur:
                        n = kb // 2
                        MM(pst.t[:], Ec.t[:, n * 128:(n + 1) * 128], mbT.t[:], True, False, [Ec.b, mbT.b], [pst.b], skip=True)
                        first = False
                    elif kb == i:
                        MM(pst.t[:], ident_bf.t[:], tri4.t[:], True, False, [ident_bf.b, tri4.b], [pst.b], skip=True)
                        first = False
                    for h in range(4):
                        p0 = (h % 2) * 64
                        MM(pst.t[:, h * 128:(h + 1) * 128], KBt.t[:, h // 2, ks], QB.t[:, h, :],
                           first and h == 0, h == 3, [KBt.b, QB.b], [pst.b], skip=True)
                    pT = pT_r.next()
                    ACT(pT.t[:], pst.t[:], AF.Exp, [pst.b], [pT.b], scale=0.125)
                    for h in range(4):
                        MM(OB.t[:, h * 65:(h + 1) * 65], pT.t[:, h * 128:(h + 1) * 128], VBt.t[:, kb, h * 65:(h + 1) * 65],
                           kb == 0 and h == 0, kb == i and h == 3, [pT.b, VBt.b], [OB.b], skip=True)
                OA = PO.next()
                for kb in range(i + 1 if AP_ & 8 else 0):
                    ks = slice(kb * 128, (kb + 1) * 128)
                    pst = PS.next()
                    MM(pst.t[:], mb.t[:, ks], i4.t[:], True, False, [mb.b, i4.b], [pst.b])
                    MM(pst.t[:], KI.t[:, ks], QAz.t[:].rearrange("p h q -> p (h q)"), False, True, [ka_b, ik_b, QAz.b], [pst.b])
                    pT = pT_r.next()
                    ACT(pT.t[:], pst.t[:], AF.Exp, [pst.b], [pT.b], scale=0.125)
                    for h in range(4):
                        MM(OA.t[:, h * 65:(h + 1) * 65], pT.t[:, h * 128:(h + 1) * 128], VA.t[:, kb, :],
                           kb == 0 and h == 0, kb == i and h == 3, [pT.b, VA.b], [OA.b], skip=True)
                oab = oab_r.next()
                for (O_, c0, r0) in (((OA, 0, 0), (OB, 256, 4)) if AP_ & 16 else ()):
                    ov = O_.t[:, 0:260].rearrange("p (h d) -> p h d", d=65)
                    RECIP(rec.t[:, r0:r0 + 4], ov[:, :, 64], [O_.b], [rec.b])
                    for h in range(4):
                        TS("dve", oab.t[:, c0 + h * 64:c0 + (h + 1) * 64], ov[:, h, 0:64], rec.t[:, r0 + h:r0 + h + 1], ALU.mult,
                           [O_.b, rec.b], [oab.b])
                DMA("sp", OAB_d[i * 128:(i + 1) * 128, :], oab.t[:], [oab.b], [db("OAB", i)])
            S.barrier()

        if "A2" in stages:
            A.reset(base_mark)
            wbr = A.tile("wbr", [128, 8, D], BF16)
            wo = A.tile("wo", [128, 8, D], BF16)
            g1 = A.tile("g1", [128, D], F32)
            b1 = A.tile("b1", [128, D], F32)
            esink = A.tile("esink", [128, 8], F32)
            QC_r = A.ring("QC", [128, 2, 4, 128], BF16, 2)
            import os
            for tl in (QC_r.items if not int(os.environ.get("A2SKIP", "0")) & 8 else []):
                MSET("dve", tl.t[:], 0.0, [tl.b])
            KC_r = [A.tile(f"KC{k}", [128, 128], BF16) for k in range(3)]
            VC_r = [A.tile(f"VC{k}", [128, 130], BF16) for k in range(3)]
            gT_r = A.ring("gT", [128, 24, 128], BF16, 2)
            xb_r2 = A.ring("xblk", [128, D], F32, 2)
            oall_r = A.ring("oall", [128, D], BF16, 2)
            oT_r = A.ring("oT", [128, 8, 128], BF16, 2)
            tmp_r = A.ring("mtmp", [128, 3, 128], F32, 2)
            mT_r = A.ring("mT", [128, 8, 128], BF16, 2)
            r_r = A.ring("r", [128, D], F32, 2)
            xo_r = A.ring("xo", [128, D], F32, 2)
            pT_r = A.ring("pT2", [128, 512], BF16, 3)
            rec = A.tile("rec2", [128, 8], F32)
            lnt = {"st": A.tile("st", [128, 2, 6], F32), "mv": A.tile("mv", [128, 2], F32), "sd": A.tile("sd", [128, 4], F32)}
            import os
            SK = int(os.environ.get("A2SKIP", "0"))
            if not SK & 1:
                DMA("pool", wbr.t[:], wbr_d[l].rearrange("(kc p) n -> p kc n", p=128), [], [wbr.b])
                DMA("pool", wo.t[:], wo_d[l].rearrange("(kc p) n -> p kc n", p=128), [], [wo.b])
            if not SK & 2:
                DMA("sp", g1.t[:], lnp_d[l, 0], [], [g1.b])
                DMA("sp", b1.t[:], lnp_d[l, 1], [], [b1.b])
                DMA("sp", esink.t[:], sinks_d[l], [], [esink.b])
            if not SK & 4:
                ACT(esink.t[:], esink.t[:], AF.Exp, [esink.b], [esink.b])

            def load_a2(i):
                bsl = slice(i * 128, (i + 1) * 128)
                g = i // 4
                QC = QC_r.next()
                gT = gT_r.next()
                xblk = xb_r2.next()
                oall = oall_r.next()
                KC = KC_r[i % 3]
                VC = VC_r[i % 3]
                oc_b = Buf("oall_c")
                for gq in range(2):
                    DMA("sp", QC.t[gq * 64:(gq + 1) * 64, gq, :, :], QC_d[gq * 64:(gq + 1) * 64, :, bsl], [db("QC", g)], [QC.b])
                DMA("sp", KC.t[:], KC_d[:, bsl], [db("KC", g)], [KC.b])
                DMA("sp", VC.t[:], VC_d[bsl, :], [db("VC", i)], [VC.b])
                DMA("sp", gT.t[:], GT_d[:, :, bsl], [db("GT", g)], [gT.b])
                DMA("sp", xblk.t[:], xin_d[bsl, :], [db(xin_name, i)], [xblk.b])
                DMA("sp", oall.t[:, 0:512], OAB_d[bsl, :], [db("OAB", i), oc_b], [oall.b])
                return QC, gT, xblk, oall, oc_b

            import os
            NB2 = int(os.environ.get("A2NB", str(NB)))
            nxt = load_a2(0) if NB2 > 0 else None
            for i in range(NB2):
                QC, gT, xblk, oall, oc_b = nxt
                if i + 1 < NB2:
                    nxt = load_a2(i + 1)
                for gq in range(2):
                    OC = PO.next()
                    kbs = ([i - 1] if i > 0 else []) + [i]
                    for kb in kbs:
                        KC = KC_r[kb % 3]
                        VC = VC_r[kb % 3]
                        pst = PS.next()
                        mk = band4 if kb == i - 1 else tri4
                        MM(pst.t[:], ident_bf.t[:], mk.t[:], True, False, [ident_bf.b, mk.b], [pst.b])
                        MM(pst.t[:], KC.t[:, :], QC.t[:, gq, :, :].rearrange("p h q -> p (h q)"),
                           False, True, [KC.b, QC.b], [pst.b])
                        pT = pT_r.next()
                        ACT(pT.t[:], pst.t[:], AF.Exp, [pst.b], [pT.b], scale=0.125)
                        for r in range(4):
                            MM(OC.t[:, r * 65:(r + 1) * 65], pT.t[:, r * 128:(r + 1) * 128], VC.t[:, gq * 65:(gq + 1) * 65],
                               kb == kbs[0] and r == 0, kb == i and r == 3, [pT.b, VC.b], [OC.b], skip=True)
                    ov = OC.t[:, 0:260].rearrange("p (h d) -> p h d", d=65)
                    TT("dve", rec.t[:, 0:4], ov[:, :, 64], esink.t[:, gq * 4:(gq + 1) * 4], ALU.add, [OC.b, esink.b], [rec.b])
                    RECIP(rec.t[:, 4:8], rec.t[:, 0:4], [rec.b], [rec.b])
                    for r in range(4):
                        c0 = 512 + (gq * 4 + r) * 64
                        TS("dve", oall.t[:, c0:c0 + 64], ov[:, r, 0:64], rec.t[:, 4 + r:5 + r], ALU.mult, [OC.b, rec.b], [oc_b])
                pt = PS.next()
                ptv = pt.t[:].bitcast(BF16).rearrange("p (k q) -> p k q", k=8)
                for kc in range(8):
                    TR(ptv[:, kc, :], oall.t[:, kc * 128:(kc + 1) * 128], ident_bf.t[:], [oall.b, oc_b, ident_bf.b], [pt.b])
                oT = oT_r.next()
                CP("act", oT.t[:], ptv, [pt.b], [oT.b])
                if debug and l == 0:
                    DMA("sp", DBGo_d[i * 128:(i + 1) * 128, :], oall.t[:], [oall.b, oc_b], [db("DBGo", i)])
                mT = mT_r.next()
                for m in range(8):
                    pm = PS.next()
                    ms = slice(m * 128, (m + 1) * 128)
                    for (br, kcs) in ((0, (0, 1)), (1, (2, 3)), (2, (4, 5, 6, 7))):
                        for kc in kcs:
                            MM(pm.t[:, br * 128:(br + 1) * 128], wbr.t[:, kc, ms], oT.t[:, kc, :], kc == 0, kc == 7,
                               [wbr.b, oT.b], [pm.b], skip=True)
                    tmp = tmp_r.next()
                    TT("dve", tmp.t[:], pm.t[:, 0:384].rearrange("p (b q) -> p b q", b=3), gT.t[:, m:24:8, :], ALU.mult, [pm.b, gT.b], [tmp.b])
                    RED(mT.t[:, m, :], tmp.t[:].rearrange("p b q -> p q b"), ALU.add, [tmp.b], [mT.b])
                r_ = r_r.next()
                for nh in range(2):
                    py = PS.next()
                    ns = slice(nh * 512, (nh + 1) * 512)
                    for m in range(8):
                        MM(py.t[:], mT.t[:, m, :], wo.t[:, m, ns], m == 0, m == 7, [mT.b, wo.b], [py.b])
                    STT(r_.t[:, ns], xblk.t[:, ns], DN_ALPHA, py.t[:], ALU.mult, ALU.add, [xblk.b, py.b], [r_.b])
                if debug and l == 0:
                    DMA("sp", DBGm_d[:, :, i * 128:(i + 1) * 128], mT.t[:], [mT.b], [db("DBGm", i)])
                    DMA("sp", DBGr_d[i * 128:(i + 1) * 128, :], r_.t[:], [r_.b], [db("DBGr", i)])
                xo = xo_r.next()
                layer_norm(r_, g1, b1, xo, lnt)
                DMA("sp", X1_d[i * 128:(i + 1) * 128, :], xo.t[:], [xo.b], [db("X1", i)])
            S.barrier()

        if "M" in stages:
            A.reset(base_mark)
            NQB = 16
            accs = [A.tile(f"macc{k}", [128, D], F32) for k in range(NQB)]
            x1T = A.tile("x1T", [128, 8, NQB * 128], BF16)
            W_r = A.ring("mw", [128, 4096], BF16, 5)
            hT_r = A.ring("hT", [128, 4, 512], BF16, 2)
            sg_r = A.ring("sg", [128, 512], BF16, 2)
            x1f_r = A.ring("x1f", [128, D], F32, 2)
            x1Tf_r = A.ring("x1Tf", [128, 8, 128], F32, 2)
            cw = A.tile("cw", [128, NQB, NE], F32)
            g2 = A.tile("g2", [128, D], F32)
            b2 = A.tile("b2", [128, D], F32)
            xo_r = A.ring("xo2", [128, D], F32, 2)
            lnt = {"st": A.tile("st2", [128, 2, 6], F32), "mv": A.tile("mv2", [128, 2], F32), "sd": A.tile("sd2", [128, 4], F32)}
            rt = {k: A.tile(f"rt_{k}", [128, NE], F32) for k in ("aff", "sel", "selm", "sel2", "e1", "e2")}
            rs = A.tile("rsm", [128, 16], F32)
            DMA("sp", g2.t[:], lnp_d[l, 2], [], [g2.b])
            DMA("sp", b2.t[:], lnp_d[l, 3], [], [b2.b])
            for qt in range(S_LEN // (NQB * 128)):
                for k in range(NQB):
                    blk = qt * NQB + k
                    x1f = x1f_r.next()
                    DMA("sp", x1f.t[:], X1_d[blk * 128:(blk + 1) * 128, :], [db("X1", blk)], [x1f.b])
                    x1Tf = x1Tf_r.next()
                    for half in range(2):
                        pt = PS.next()
                        for c in range(4):
                            kc = half * 4 + c
                            TR(pt.t[:, c * 128:(c + 1) * 128], x1f.t[:, kc * 128:(kc + 1) * 128], ident_f.t[:], [x1f.b, ident_f.b], [pt.b])
                        CP("act", x1Tf.t[:, half * 4:(half + 1) * 4, :], pt.t[:].rearrange("p (c q) -> p c q", c=4), [pt.b], [x1Tf.b])
                        CP("dve", x1T.t[:, half * 4:(half + 1) * 4, k * 128:(k + 1) * 128], pt.t[:].rearrange("p (c q) -> p c q", c=4), [pt.b], [x1T.b])
                    ACT(accs[k].t[:], x1f.t[:], AF.Copy, [x1f.b], [accs[k].b], scale=DN_ALPHA)
                    pr = PS.next()
                    for kc in range(8):
                        MM(pr.t[:, 0:NE], x1Tf.t[:, kc, :], rw.t[:, kc, :], kc == 0, kc == 7, [x1Tf.b, rw.b], [pr.b])
                    aff, sel, selm, sel2, e1, e2 = (rt[n] for n in ("aff", "sel", "selm", "sel2", "e1", "e2"))
                    ACT(aff.t[:], pr.t[:, 0:NE], AF.Sigmoid, [pr.b], [aff.b])
                    TT("dve", sel.t[:], aff.t[:], rb.t[:], ALU.add, [aff.b, rb.b], [sel.b])
                    sel3 = sel.t[:].rearrange("p (g e) -> p g e", g=4)
                    RED(rs.t[:, 0:4], sel3, ALU.max, [sel.b], [rs.b])
                    for gq in range(4):
                        TS("dve", e1.t[:, gq * 4:(gq + 1) * 4], sel.t[:, gq * 4:(gq + 1) * 4], rs.t[:, gq:gq + 1], ALU.is_equal,
                           [sel.b, rs.b], [e1.b])
                    STT(sel2.t[:], e1.t[:], -BIG, sel.t[:], ALU.mult, ALU.add, [e1.b, sel.b], [sel2.b])
                    RED(rs.t[:, 4:8], sel2.t[:].rearrange("p (g e) -> p g e", g=4), ALU.max, [sel2.b], [rs.b])
                    TT("dve", rs.t[:, 8:12], rs.t[:, 0:4], rs.t[:, 4:8], ALU.add, [rs.b], [rs.b])
                    RED(rs.t[:, 12:13], rs.t[:, 8:12], ALU.max, [rs.b], [rs.b])
                    TS("dve", rs.t[:, 8:12], rs.t[:, 8:12], rs.t[:, 12:13], ALU.is_equal, [rs.b], [rs.b])
                    TS("dve", rs.t[:, 8:12], rs.t[:, 8:12], -1.0, ALU.add, [rs.b], [rs.b], s2=BIG, op1=ALU.mult)
                    for gq in range(4):
                        TS("dve", selm.t[:, gq * 4:(gq + 1) * 4], sel.t[:, gq * 4:(gq + 1) * 4], rs.t[:, 8 + gq:9 + gq], ALU.add,
                           [sel.b, rs.b], [selm.b])
                    RED(rs.t[:, 13:14], selm.t[:], ALU.max, [selm.b], [rs.b])
                    TS("dve", e1.t[:], selm.t[:], rs.t[:, 13:14], ALU.is_equal, [selm.b, rs.b], [e1.b])
                    STT(sel2.t[:], e1.t[:], -BIG, selm.t[:], ALU.mult, ALU.add, [e1.b, selm.b], [sel2.b])
                    RED(rs.t[:, 14:15], sel2.t[:], ALU.max, [sel2.b], [rs.b])
                    TS("dve", e2.t[:], sel2.t[:], rs.t[:, 14:15], ALU.is_equal, [sel2.b, rs.b], [e2.b])
                    TT("dve", e1.t[:], e1.t[:], e2.t[:], ALU.add, [e1.b, e2.b], [e1.b])
                    TT("dve", e1.t[:], e1.t[:], aff.t[:], ALU.mult, [e1.b, aff.b], [e1.b])
                    RED(rs.t[:, 15:16], e1.t[:], ALU.add, [e1.b], [rs.b])
                    RECIP(rs.t[:, 15:16], rs.t[:, 15:16], [rs.b], [rs.b])
                    TS("dve", cw.t[:, k, :], e1.t[:], rs.t[:, 15:16], ALU.mult, [e1.b, rs.b], [cw.b])
                for e in range(NE):
                    wg = W_r.next()
                    wu = W_r.next()
                    wd = W_r.next()
                    DMA("pool", wg.t[:].rearrange("p (k n) -> p k n", k=8), wg_d[l, e].rearrange("(kc p) n -> p kc n", p=128), [], [wg.b])
                    DMA("pool", wu.t[:].rearrange("p (k n) -> p k n", k=8), wu_d[l, e].rearrange("(kc p) n -> p kc n", p=128), [], [wu.b])
                    DMA("pool", wd.t[:].rearrange("p (k n) -> p k n", k=4), wd_d[l, e].rearrange("(kc p) n -> p kc n", p=128), [], [wd.b])
                    wgv = wg.t[:].rearrange("p (k n) -> p k n", k=8)
                    wuv = wu.t[:].rearrange("p (k n) -> p k n", k=8)
                    wdv = wd.t[:].rearrange("p (k n) -> p k n", k=4)
                    for tg in range(NQB // 4):
                        tsl = slice(tg * 512, (tg + 1) * 512)
                        hT = hT_r.next()
                        for mc in range(4):
                            msl = slice(mc * 128, (mc + 1) * 128)
                            pg = PS.next()
                            pu = PS.next()
                            for kc in range(8):
                                MM(pg.t[:], wgv[:, kc, msl], x1T.t[:, kc, tsl], kc == 0, kc == 7, [wg.b, x1T.b], [pg.b])
                            for kc in range(8):
                                MM(pu.t[:], wuv[:, kc, msl], x1T.t[:, kc, tsl], kc == 0, kc == 7, [wu.b, x1T.b], [pu.b])
                            sg = sg_r.next()
                            ACT(sg.t[:], pg.t[:], AF.Silu, [pg.b], [sg.b])
                            TT("dve", hT.t[:, mc, :], sg.t[:], pu.t[:], ALU.mult, [sg.b, pu.b], [hT.b])
                        for tb in range(4):
                            k = tg * 4 + tb
                            for nh in range(2):
                                ns = slice(nh * 512, (nh + 1) * 512)
                                py = PS.next()
                                for mc in range(4):
                                    MM(py.t[:], hT.t[:, mc, tb * 128:(tb + 1) * 128], wdv[:, mc, ns], mc == 0, mc == 3, [hT.b, wd.b], [py.b])
                                STT(accs[k].t[:, ns], py.t[:], cw.t[:, k, e:e + 1], accs[k].t[:, ns], ALU.mult, ALU.add,
                                    [py.b, cw.b, accs[k].b], [accs[k].b])
                for k in range(NQB):
                    blk = qt * NQB + k
                    xo = xo_r.next()
                    layer_norm(accs[k], g2, b2, xo, lnt)
                    if l == depth - 1:
                        out_ops.append(DMA("sp", out_d[blk * 128:(blk + 1) * 128, :], xo.t[:], [xo.b], [db("out", blk)]))
                    else:
                        DMA("sp", XL_d[blk * 128:(blk + 1) * 128, :], xo.t[:], [xo.b], [db("XL", blk)])
            S.barrier()

    if not out_ops:
        out_ops = list(S.dma_last.values())
    S.emit(out_ops)
    global _LAST_S
    _LAST_S = S
    return nc


def _bf(a):
    return np.asarray(a, dtype=np.float32).astype(ml_dtypes.bfloat16)


def _consts():
    c = {}
    eye = np.eye(128, dtype=np.float32)
    c["c_ident_bf"] = _bf(eye)
    c["c_ident_f"] = eye
    c["c_i4"] = _bf(np.tile(eye, (1, 4)))
    key = np.arange(128)[:, None]
    q = np.arange(128)[None, :]
    tri = np.where(key > q, NEG, 0.0).astype(np.float32)
    band = np.where(key <= q, NEG, 0.0).astype(np.float32)
    c["c_tri4"] = _bf(np.tile(tri, (1, 4)))
    c["c_band4"] = _bf(np.tile(band, (1, 4)))
    c["c_trineg"] = np.where(q > key, -BIG, 0.0).astype(np.float32)
    E = np.zeros((32, 4096), np.float32)
    for n in range(32):
        E[n, n * 128:(n + 1) * 128] = 1.0
    c["c_E"] = _bf(E)
    c["c_pow"] = np.tile((0.5 ** np.arange(1, NBIS + 1, dtype=np.float64)).astype(np.float32)[None, :], (128, 1))
    inv = 1.0 / (10000.0 ** (np.arange(0, 64, 2, dtype=np.float32) / np.float32(64)))
    ang = np.arange(S_LEN, dtype=np.float32)[:, None] * inv[None, :].astype(np.float32)
    cos = np.cos(ang).astype(np.float32)
    sin = np.sin(ang).astype(np.float32)
    p = np.arange(128)
    cosT = cos[:, p % 32].T
    sgn = np.where((p % 64) < 32, -1.0, 1.0).astype(np.float32)
    sinT = (sin[:, p % 32].T) * sgn[:, None]
    c["cosT"] = np.ascontiguousarray(cosT, dtype=np.float32)
    c["sinT"] = np.ascontiguousarray(sinT, dtype=np.float32)
    return c


def _col_plan():
    offs = np.concatenate([[0], np.cumsum(IN_SIZES)])
    o_aq, o_ac, o_iq, o_ik, o_iw, o_bq, o_bk, o_bv, o_cq, o_ck, o_cv, o_g = offs[:12]

    def heads(c0, h0, nh):
        main, swp = [], []
        for h in range(h0, h0 + nh):
            for d in range(64):
                main.append(c0 + h * 64 + d)
                swp.append(c0 + h * 64 + (d + 32) % 64)
        return main, swp

    units = []
    units += [heads(o_aq, 0, 2), heads(o_aq, 2, 2)]
    units += [heads(o_iq, 0, 2), heads(o_iq, 2, 2)]
    m, s = heads(o_ik, 0, 1)
    units += [(m + m, s + s)]
    units += [heads(o_bq, 0, 2), heads(o_bq, 2, 2)]
    units += [heads(o_bk, 0, 2), heads(o_bk, 2, 2)]
    units += [heads(o_cq, 2 * c, 2) for c in range(4)]
    units += [heads(o_ck, 0, 2)]
    chunks = []
    for (m, s) in units:
        chunks.append(np.array(m))
        chunks.append(np.array(s))
    chunks.append(np.arange(o_ac, o_ac + 128))
    for j in range(24):
        chunks.append(np.arange(o_g + 128 * j, o_g + 128 * (j + 1)))
    assert len(chunks) == NFM
    tm = np.concatenate([np.arange(o_bv, o_bv + 256), np.arange(o_cv, o_cv + 128), np.arange(o_iw, o_iw + 4)])
    return chunks, tm


def _prep_shared(inp):
    f = lambda a: np.ascontiguousarray(np.asarray(a, dtype=np.float32))
    w_in = f(inp["w_in"])
    chunks, tm = _col_plan()
    w_fm = np.empty((DEPTH, NFM, 128, 8, 128), np.float32)
    w_tm = np.empty((DEPTH, 128, 8, NTM), np.float32)
    for l in range(DEPTH):
        wl = w_in[l].reshape(8, 128, N_IN)
        for ci, idx in enumerate(chunks):
            w_fm[l, ci] = wl[:, :, idx].transpose(1, 0, 2)
        w_tm[l] = wl[:, :, tm].transpose(1, 0, 2)
    sw = np.array([(d + 32) % 64 for d in range(64)])
    uk = f(inp["a_w_uk"])
    sh = {
        "w_fm": w_fm, "w_tm": w_tm, "uk": uk, "uksw": np.ascontiguousarray(uk[:, :, sw]), "uv": f(inp["a_w_uv"]),
        "w_branch": f(inp["w_branch"]), "w_o": f(inp["w_o"]),
        "moe_w_gate": f(inp["moe_w_gate"]), "moe_w_up": f(inp["moe_w_up"]), "moe_w_down": f(inp["moe_w_down"]),
    }
    lnp = np.empty((DEPTH, 4, 128, D), np.float32)
    for l in range(DEPTH):
        for k, nm in enumerate(("ln1_g", "ln1_b", "ln2_g", "ln2_b")):
            lnp[l, k] = np.broadcast_to(f(inp[nm])[l][None, :], (128, D))
    sh["lnp"] = lnp
    sh["sinks_bc"] = np.ascontiguousarray(np.broadcast_to(f(inp["c_sinks"])[:, None, :], (DEPTH, 128, 8)))
    sh["rw"] = np.ascontiguousarray(f(inp["router_w"]).reshape(8, 128, NE).transpose(1, 0, 2))
    sh["rb_bc"] = np.ascontiguousarray(np.broadcast_to(f(inp["router_b"])[None, :], (128, NE)))
    sh.update(_consts())
    return sh


_NC_CACHE = {}


def kernel(**inputs):
    x = np.ascontiguousarray(np.asarray(inputs["x"], dtype=np.float32))
    sh = _prep_shared(inputs)
    if "nc" not in _NC_CACHE:
        _NC_CACHE["nc"] = build_program()
    nc = _NC_CACHE["nc"]
    in_maps = []
    for b in range(NCORES):
        m = dict(sh)
        m["x"] = x[b]
        in_maps.append(m)
    res = run_bass_kernel_spmd(nc, in_maps, core_ids=list(range(NCORES)))
    out = np.stack([np.asarray(res.results[b]["out"], dtype=np.float32) for b in range(NCORES)], axis=0)
    return out
```

```python
import bisect
import math
import numpy as np
import ml_dtypes
import concourse.bass as bass
import concourse.mybir as mybir
from concourse.bass_utils import run_bass_kernel_spmd

F32 = mybir.dt.float32
BF16 = mybir.dt.bfloat16
AF = mybir.ActivationFunctionType
ALU = mybir.AluOpType
AX = mybir.AxisListType

NCORES = 4
S_LEN = 8192
D = 1024
NB = S_LEN // 128
NG = S_LEN // 512
DEPTH = 2
IN_SIZES = (256, 128, 256, 64, 4, 256, 256, 256, 512, 128, 128, 3072)
N_IN = sum(IN_SIZES)
NFM = 53
NTM = 388
DN_ALPHA = (2 * DEPTH) ** 0.25
LN_EPS = 1e-5
NEG = -30000.0
BIG = 1.0e30
TOPK = 256
NBIS = 14
NE = 16
DE = 512


class Op:
    __slots__ = ("eng", "fn", "waits", "sig", "idx", "dma_sem")

    def __init__(self, eng, fn):
        self.eng = eng
        self.fn = fn
        self.waits = []
        self.sig = None
        self.idx = None
        self.dma_sem = None


class Buf:
    __slots__ = ("name", "lw", "rd", "excl")

    def __init__(self, name="", excl=False):
        self.name = name
        self.lw = None
        self.rd = []
        self.excl = excl


class T:
    __slots__ = ("t", "b")

    def __init__(self, t, b):
        self.t = t
        self.b = b


class Ring:
    def __init__(self, items):
        self.items = items
        self.i = 0

    def next(self):
        it = self.items[self.i % len(self.items)]
        self.i += 1
        return it


class Sched:
    ENGS = ("pe", "act", "dve", "pool", "sp")

    def __init__(self, nc, n_dma_sems=16):
        self.nc = nc
        self.ops = {e: [] for e in self.ENGS}
        self.count = {e: 0 for e in self.ENGS}
        self.sig_idx = {e: [] for e in self.ENGS}
        self.known = {e: {} for e in self.ENGS}
        self.n_dma_sems = n_dma_sems
        self.dma_cnt = {}
        self.dma_rr = {e: 0 for e in self.ENGS}
        self.dma_last = {}
        self.last_real = {e: None for e in self.ENGS}

    def ticket(self, op):
        if op.dma_sem is not None:
            return op.dma_sem
        e = op.eng
        if op.sig is not None:
            return (e, op.sig)
        lst = self.sig_idx[e]
        j = bisect.bisect_left(lst, op.idx)
        if j < len(lst):
            return (e, self.ops[e][lst[j]].sig)
        self.count[e] += 1
        op.sig = self.count[e]
        lst.append(op.idx)
        return (e, op.sig)

    def need(self, op, dep):
        if dep is None:
            return
        if dep.eng == "pe" and op.eng == "pe" and dep.dma_sem is None and op.dma_sem is None:
            return
        import os as _o
        if _o.environ.get("NOSELF") == "1" and dep.eng == op.eng and dep.dma_sem is None and op.dma_sem is None:
            return
        k, v = self.ticket(dep)
        kn = self.known[op.eng]
        if kn.get(k, 0) >= v:
            return
        kn[k] = v
        op.waits.append((k, v))

    def add(self, eng, fn, reads=(), writes=(), dma=False):
        op = Op(eng, fn)
        op.idx = len(self.ops[eng])
        if dma:
            op.dma_sem = ("pending", 0)
        for b in reads:
            self.need(op, b.lw)
            if b.excl:
                last = {}
                for r in b.rd:
                    if r.eng != eng and (r.eng not in last or r.idx > last[r.eng].idx):
                        last[r.eng] = r
                for r in last.values():
                    self.need(op, r)
        for b in writes:
            self.need(op, b.lw)
            last = {}
            for r in b.rd:
                if r.dma_sem is not None:
                    self.need(op, r)
                elif r.eng not in last or r.idx > last[r.eng].idx:
                    last[r.eng] = r
            for r in last.values():
                self.need(op, r)
        if dma:
            j = self.dma_rr[eng]
            self.dma_rr[eng] = (j + 1) % self.n_dma_sems
            key = ("dma", eng, j)
            prev = self.dma_last.get(key)
            if prev is not None:
                self.need(op, prev)
            self.dma_cnt[key] = self.dma_cnt.get(key, 0) + 1
            op.dma_sem = (key, 16 * self.dma_cnt[key])
            self.dma_last[key] = op
        self.ops[eng].append(op)
        self.last_real[eng] = op
        for b in writes:
            b.lw = op
            b.rd = []
        for b in reads:
            b.rd.append(op)
        return op

    def barrier(self):
        deps = [o for o in self.last_real.values() if o is not None and o.dma_sem is None]
        deps += list(self.dma_last.values())
        for e in self.ENGS:
            op = Op(e, None)
            op.idx = len(self.ops[e])
            for d in deps:
                if d.eng == e and d.dma_sem is None:
                    continue
                self.need(op, d)
            self.ops[e].append(op)

    def emit(self, final_ops):
        nc = self.nc
        fin = Op("sp", None)
        fin.idx = len(self.ops["sp"])
        for o in final_ops:
            self.need(fin, o)
        self.ops["sp"].append(fin)
        semkeys = [e for e in self.ENGS if self.count[e] > 0] + list(self.dma_cnt.keys())
        sems = {}
        for i, k in enumerate(semkeys):
            sems[k] = nc.alloc_semaphore(name=f"sem{i}")
        with nc.allow_low_precision(reason="bf16 matmul operands, fp32 accumulation"), nc.Block() as block:
            def mk(e):
                def body(h):
                    for op in self.ops[e]:
                        for (k, v) in op.waits:
                            h.wait_ge(sems[k], v)
                        if op.fn is None:
                            continue
                        inst = op.fn(h)
                        if op.dma_sem is not None:
                            inst.then_inc(sems[op.dma_sem[0]], 16)
                        elif op.sig is not None:
                            inst.then_inc(sems[e], 1)
                return body
            block.tensor(mk("pe"))
            block.scalar(mk("act"))
            block.vector(mk("dve"))
            block.gpsimd(mk("pool"))
            block.sync(mk("sp"))


class Alloc:
    def __init__(self, nc, base, top):
        self.nc = nc
        self.cur = base
        self.top = top
        self.n = 0

    def mark(self):
        return self.cur

    def reset(self, m):
        self.cur = m

    def tile(self, name, shape, dtype, nbuf=None):
        size = 2 if dtype == BF16 else 4
        nbytes = size
        for s in shape[1:]:
            nbytes *= s
        off = (self.cur + 63) // 64 * 64
        assert off + nbytes <= self.top, f"SBUF overflow allocating {name}: {off}+{nbytes} > {self.top}"
        self.n += 1
        t = self.nc.alloc_sbuf_tensor_at(f"{name}_{self.n}", list(shape), dtype, offset=off)
        self.cur = off + nbytes
        return T(t, Buf(name))

    def ring(self, name, shape, dtype, n):
        return Ring([self.tile(f"{name}{i}", shape, dtype) for i in range(n)])


def build_program(debug=False, stages=("P", "A1", "A2", "M"), depth=DEPTH):
    nc = bass.Bass("TRN2", target_bir_lowering=False)
    import os as _os
    S = Sched(nc, n_dma_sems=int(_os.environ.get("NDS", "16")))

    def din(name, shape, dt=F32):
        return nc.dram_tensor(name, list(shape), dt, kind="ExternalInput").ap()

    scr_kind = "ExternalOutput" if debug else "Internal"

    def dscr(name, shape, dt):
        return nc.dram_tensor(name, list(shape), dt, kind=scr_kind).ap()

    x_d = din("x", [S_LEN, D])
    wfm_d = din("w_fm", [DEPTH, NFM, 128, 8, 128])
    wtm_d = din("w_tm", [DEPTH, 128, 8, NTM])
    uk_d = din("uk", [DEPTH, 128, 64])
    uksw_d = din("uksw", [DEPTH, 128, 64])
    uv_d = din("uv", [DEPTH, 128, 64])
    wbr_d = din("w_branch", [DEPTH, D, D])
    wo_d = din("w_o", [DEPTH, D, D])
    lnp_d = din("lnp", [DEPTH, 4, 128, D])
    sinks_d = din("sinks_bc", [DEPTH, 128, 8])
    rw_d = din("rw", [128, 8, NE])
    rb_d = din("rb_bc", [128, NE])
    wg_d = din("moe_w_gate", [DEPTH, NE, D, DE])
    wu_d = din("moe_w_up", [DEPTH, NE, D, DE])
    wd_d = din("moe_w_down", [DEPTH, NE, DE, D])
    cos_d = din("cosT", [128, S_LEN])
    sin_d = din("sinT", [128, S_LEN])
    c_ident_bf = din("c_ident_bf", [128, 128], BF16)
    c_ident_f = din("c_ident_f", [128, 128])
    c_i4 = din("c_i4", [128, 512], BF16)
    c_tri4 = din("c_tri4", [128, 512], BF16)
    c_band4 = din("c_band4", [128, 512], BF16)
    c_trineg = din("c_trineg", [128, 128])
    c_E = din("c_E", [32, 4096], BF16)
    c_pow = din("c_pow", [128, NBIS])
    out_d = nc.dram_tensor("out", [S_LEN, D], F32, kind="ExternalOutput").ap()

    QA_d = dscr("QA_d", [64, 4, S_LEN], BF16)
    IQ_d = dscr("IQ_d", [64, 4, S_LEN], BF16)
    IK_d = dscr("IK_d", [64, S_LEN], BF16)
    KA_d = dscr("KA_d", [64, S_LEN], BF16)
    QB_d = dscr("QB_d", [128, 2, S_LEN], BF16)
    KB_d = dscr("KB_d", [128, 2, S_LEN], BF16)
    QC_d = dscr("QC_d", [128, 4, S_LEN], BF16)
    KC_d = dscr("KC_d", [128, S_LEN], BF16)
    GT_d = dscr("GT_d", [128, 24, S_LEN], BF16)
    VA_d = dscr("VA_d", [128, NB, 65], BF16)
    VB_d = dscr("VB_d", [128, NB, 260], BF16)
    VC_d = dscr("VC_d", [S_LEN, 130], BF16)
    IW_d = dscr("IW_d", [S_LEN, 8], F32)
    OAB_d = dscr("OAB_d", [S_LEN, 512], BF16)
    X1_d = dscr("X1_d", [S_LEN, D], F32)
    XL_d = dscr("XL_d", [S_LEN, D], F32)
    if debug:
        DBGo_d = dscr("DBGo_d", [S_LEN, D], BF16)
        DBGm_d = dscr("DBGm_d", [128, 8, S_LEN], BF16)
        DBGr_d = dscr("DBGr_d", [S_LEN, D], F32)
    DB = {}

    def db(name, i):
        k = (name, i)
        if k not in DB:
            DB[k] = Buf(f"{name}{i}")
        return DB[k]

    def dbg_all(name):
        return [db(name, g) for g in range(NG)]

    def MM(out, lhsT, rhs, start, stop, R, W, skip=False):
        S.add("pe", lambda h: h.matmul(out, lhsT=lhsT, rhs=rhs, start=start, stop=stop, skip_group_check=skip), reads=R, writes=W)

    def TR(out, in_, ident, R, W):
        S.add("pe", lambda h: h.transpose(out=out, in_=in_, identity=ident), reads=R, writes=W)

    def ACT(out, in_, func, R, W, scale=1.0, bias=None, accum=None):
        if bias is None:
            S.add("act", lambda h: h.activation(out=out, in_=in_, func=func, scale=scale), reads=R, writes=W)
        else:
            S.add("act", lambda h: h.activation(out=out, in_=in_, func=func, scale=scale, bias=bias), reads=R, writes=W)

    def TS(eng, out, in0, s1, op0, R, W, s2=None, op1=None, accum=None):
        def f(h):
            kw = {}
            if op1 is not None:
                kw["op1"] = op1
            if accum is not None:
                kw["accum_out"] = accum
            return h.tensor_scalar(out=out, in0=in0, scalar1=s1, scalar2=s2, op0=op0, **kw)
        S.add(eng, f, reads=R, writes=W)

    def TT(eng, out, in0, in1, op, R, W):
        S.add(eng, lambda h: h.tensor_tensor(out=out, in0=in0, in1=in1, op=op), reads=R, writes=W)

    def STT(out, in0, scalar, in1, op0, op1, R, W):
        S.add("dve", lambda h: h.scalar_tensor_tensor(out=out, in0=in0, scalar=scalar, in1=in1, op0=op0, op1=op1), reads=R, writes=W)

    def RED(out, in_, op, R, W, axis=AX.X):
        S.add("dve", lambda h: h.tensor_reduce(out=out, in_=in_, axis=axis, op=op), reads=R, writes=W)

    def CP(eng, out, in_, R, W):
        if eng == "act":
            S.add("act", lambda h: h.activation(out=out, in_=in_, func=AF.Copy), reads=R, writes=W)
        else:
            S.add(eng, lambda h: h.tensor_copy(out=out, in_=in_), reads=R, writes=W)

    def RECIP(out, in_, R, W):
        S.add("dve", lambda h: h.reciprocal(out=out, in_=in_), reads=R, writes=W)

    def MAX8(out, in_, R, W):
        S.add("dve", lambda h: h.max(out=out, in_=in_), reads=R, writes=W)

    def MSET(eng, ap, val, W):
        S.add(eng, lambda h: h.memset(ap, val), writes=W)

    def DMA(eng, out, in_, R, W, mdl=4096):
        if eng == "pool":
            return S.add(eng, lambda h: h.dma_start(out=out, in_=in_, max_dma_last_dim=mdl), reads=R, writes=W, dma=True)
        return S.add(eng, lambda h: h.dma_start(out=out, in_=in_), reads=R, writes=W, dma=True)

    A = Alloc(nc, 16512, 229344)
    PSB = [T(nc.alloc_psum_tensor(f"psb{i}", [128, 512], F32), Buf(f"psb{i}", excl=True)) for i in range(8)]
    PS = Ring(PSB[0:6])
    PO = Ring(PSB[6:8])

    ident_bf = A.tile("ident_bf", [128, 128], BF16)
    ident_f = A.tile("ident_f", [128, 128], F32)
    i4 = A.tile("i4", [128, 512], BF16)
    tri4 = A.tile("tri4", [128, 512], BF16)
    band4 = A.tile("band4", [128, 512], BF16)
    trineg = A.tile("trineg", [128, 128], F32)
    Ec = A.tile("Ec", [32, 4096], BF16)
    cpow = A.tile("cpow", [128, NBIS], F32)
    neg1 = A.tile("neg1", [128, 1], F32)
    rw = A.tile("rw", [128, 8, NE], F32)
    rb = A.tile("rb", [128, NE], F32)
    for tl, src in ((ident_bf, c_ident_bf), (ident_f, c_ident_f), (i4, c_i4), (tri4, c_tri4), (band4, c_band4),
                    (trineg, c_trineg), (Ec, c_E), (cpow, c_pow), (rw, rw_d), (rb, rb_d)):
        DMA("sp", tl.t[:], src, [], [tl.b])
    MSET("dve", neg1.t[:], -1.0, [neg1.b])
    base_mark = A.mark()

    out_ops = []

    def layer_norm(r, g_bc, b_bc, xo, tmp):
        st, mv, sd = tmp["st"], tmp["mv"], tmp["sd"]
        S.add("dve", lambda h: h.bn_stats(out=st.t[:, 0, :], in_=r.t[:, 0:512]), reads=[r.b], writes=[st.b])
        S.add("dve", lambda h: h.bn_stats(out=st.t[:, 1, :], in_=r.t[:, 512:1024]), reads=[r.b], writes=[st.b])
        S.add("dve", lambda h: h.bn_aggr(out=mv.t[:], in_=st.t[:]), reads=[st.b], writes=[mv.b])
        TS("dve", sd.t[:, 0:1], mv.t[:, 1:2], LN_EPS, ALU.add, [mv.b], [sd.b])
        ACT(sd.t[:, 1:2], sd.t[:, 0:1], AF.Sqrt, [sd.b], [sd.b])
        S.add("dve", lambda h: h.reciprocal(out=sd.t[:, 2:3], in_=sd.t[:, 1:2]), reads=[sd.b], writes=[sd.b])
        TS("dve", r.t[:], r.t[:], mv.t[:, 0:1], ALU.subtract, [r.b, mv.b, sd.b], [r.b], s2=sd.t[:, 2:3], op1=ALU.mult)
        TT("dve", r.t[:], r.t[:], g_bc.t[:], ALU.mult, [r.b, g_bc.b], [r.b])
        TT("dve", xo.t[:], r.t[:], b_bc.t[:], ALU.add, [r.b, b_bc.b], [xo.b])

    for l in range(depth):
        xin_d = x_d if l == 0 else XL_d
        xin_name = "x" if l == 0 else "XL"

        if "P" in stages:
            A.reset(base_mark)
            Wfm = [A.tile(f"wfm{ci}", [128, 8, 128], BF16) for ci in range(NFM)]
            Wtm = A.tile("wtm", [128, 8, NTM], BF16)
            ukt = A.tile("uk", [128, 64], BF16)
            ukswt = A.tile("uksw", [128, 64], BF16)
            uvt = A.tile("uv", [128, 64], BF16)
            xf_r = A.ring("xf", [128, D], F32, 8)
            xb_r = A.ring("xb", [128, D], BF16, 2)
            xT_r = A.ring("xT", [128, 8, 512], BF16, 2)
            cs_r = A.ring("cs", [128, 512], F32, 2)
            sn_r = A.ring("sn", [128, 512], F32, 2)
            t1_r = A.ring("t1", [128, 512], F32, 2)
            t2_r = A.ring("t2", [128, 512], F32, 2)
            ost_r = A.ring("ost", [128, 512], BF16, 4)
            acT_r = A.ring("acT", [128, 512], BF16, 2)
            vast_r = A.ring("vast", [128, 65], BF16, 2)
            vbst_r = A.ring("vbst", [128, 4, 65], BF16, 2)
            vcst_r = A.ring("vcst", [128, 2, 65], BF16, 2)
            iwst_r = A.ring("iwst", [128, 8], F32, 2)
            for rg in (vast_r, vbst_r, vcst_r):
                for tl in rg.items:
                    MSET("dve", tl.t[:], 1.0, [tl.b])
            order = list(range(NFM))
            for ci in order:
                DMA("pool", Wfm[ci].t[:], wfm_d[l, ci], [], [Wfm[ci].b])
            DMA("pool", Wtm.t[:], wtm_d[l], [], [Wtm.b], mdl=NTM * 4)
            DMA("pool", ukt.t[:], uk_d[l], [], [ukt.b])
            DMA("pool", ukswt.t[:], uksw_d[l], [], [ukswt.b])
            DMA("pool", uvt.t[:], uv_d[l], [], [uvt.b])

            def load_group(g):
                res = []
                for tb in range(4):
                    blk = g * 4 + tb
                    xf = xf_r.next()
                    DMA("sp", xf.t[:], xin_d[blk * 128:(blk + 1) * 128, :], [db(xin_name, blk)], [xf.b])
                    res.append(xf)
                cs = cs_r.next()
                sn = sn_r.next()
                DMA("sp", cs.t[:], cos_d[:, g * 512:(g + 1) * 512], [], [cs.b])
                DMA("sp", sn.t[:], sin_d[:, g * 512:(g + 1) * 512], [], [sn.b])
                return res, cs, sn

            def rope_out(pa, pb_, cs, sn, np_):
                t1 = t1_r.next()
                t2 = t2_r.next()
                ost = ost_r.next()
                TT("dve", t1.t[0:np_, :], pa.t[0:np_, :], cs.t[0:np_, :], ALU.mult, [pa.b, cs.b], [t1.b])
                TT("dve", t2.t[0:np_, :], pb_.t[0:np_, :], sn.t[0:np_, :], ALU.mult, [pb_.b, sn.b], [t2.b])
                TT("dve", ost.t[0:np_, :], t1.t[0:np_, :], t2.t[0:np_, :], ALU.add, [t1.b, t2.b], [ost.b])
                return ost

            def unit_dst(u, ts):
                if u in (0, 1):
                    return [(QA_d[:, 2 * u, ts], 0, 64), (QA_d[:, 2 * u + 1, ts], 64, 128)], "QA"
                if u in (2, 3):
                    c = u - 2
                    return [(IQ_d[:, 2 * c, ts], 0, 64), (IQ_d[:, 2 * c + 1, ts], 64, 128)], "IQ"
                if u == 4:
                    return [(IK_d[:, ts], 0, 64)], "IK"
                if u in (5, 6):
                    return [(QB_d[:, u - 5, ts], 0, 128)], "QB"
                if u in (7, 8):
                    return [(KB_d[:, u - 7, ts], 0, 128)], "KB"
                if u in (9, 10, 11, 12):
                    c = u - 9
                    res = []
                    for half in range(2):
                        hh = 2 * c + half
                        gq, r = hh // 4, hh % 4
                        res.append((QC_d[gq * 64:(gq + 1) * 64, r, ts], half * 64, half * 64 + 64))
                    return res, "QC"
                return [(KC_d[:, ts], 0, 128)], "KC"

            import os
            PD = int(os.environ.get("PDBG", "15"))
            NGR = int(os.environ.get("PNG", str(NG)))
            nxt = load_group(0)
            for g in range(NGR):
                xfs, cs, sn = nxt
                if g + 1 < NGR:
                    nxt = load_group(g + 1)
                ts = slice(g * 512, (g + 1) * 512)
                xT = xT_r.next()
                for tb in range(4):
                    xb = xb_r.next()
                    CP("dve" if tb % 2 == 0 else "act", xb.t[:], xfs[tb].t[:], [xfs[tb].b], [xb.b])
                    pt = PS.next()
                    ptv = pt.t[:].bitcast(BF16).rearrange("p (k q) -> p k q", k=8)
                    for kc in range(8):
                        TR(ptv[:, kc, :], xb.t[:, kc * 128:(kc + 1) * 128], ident_bf.t[:], [xb.b, ident_bf.b], [pt.b])
                    CP("act" if tb % 2 == 0 else "dve", xT.t[:, :, tb * 128:(tb + 1) * 128], ptv, [pt.b], [xT.b])
                ulist = [int(v) for v in os.environ.get("PUNITS", ",".join(str(v) for v in range(14))).split(",")]
                for u in (ulist if PD & 1 else []):
                    pa = PS.next()
                    pb_ = PS.next()
                    for kc in range(8):
                        MM(pa.t[:], Wfm[2 * u].t[:, kc, :], xT.t[:, kc, :], kc == 0, kc == 7, [Wfm[2 * u].b, xT.b], [pa.b])
                    for kc in range(8):
                        MM(pb_.t[:], Wfm[2 * u + 1].t[:, kc, :], xT.t[:, kc, :], kc == 0, kc == 7, [Wfm[2 * u + 1].b, xT.b], [pb_.b])
                    dsts, nm = unit_dst(u, ts)
                    np_ = 64 if u == 4 else 128
                    ost = rope_out(pa, pb_, cs, sn, np_)
                    for (dap, p0, p1) in dsts:
                        DMA("sp", dap, ost.t[p0:p1, :], [ost.b], [db(nm, g)])
                if not (PD & 2):
                    continue
                pa = PS.next()
                for kc in range(8):
                    MM(pa.t[:], Wfm[28].t[:, kc, :], xT.t[:, kc, :], kc == 0, kc == 7, [Wfm[28].b, xT.b], [pa.b])
                acT = acT_r.next()
                CP("act", acT.t[:], pa.t[:], [pa.b], [acT.b])
                pa = PS.next()
                pb_ = PS.next()
                MM(pa.t[0:64, :], ukt.t[:], acT.t[:], True, True, [ukt.b, acT.b], [pa.b])
                MM(pb_.t[0:64, :], ukswt.t[:], acT.t[:], True, True, [ukswt.b, acT.b], [pb_.b])
                ost = rope_out(pa, pb_, cs, sn, 64)
                DMA("sp", KA_d[:, ts], ost.t[0:64, :], [ost.b], [db("KA", g)])
                for tb in range(4):
                    blk = g * 4 + tb
                    pv = PS.next()
                    MM(pv.t[:, 0:64], acT.t[:, tb * 128:(tb + 1) * 128], uvt.t[:], True, True, [acT.b, uvt.b], [pv.b])
                    vast = vast_r.next()
                    CP("act", vast.t[:, 0:64], pv.t[:, 0:64], [pv.b], [vast.b])
                    DMA("sp", VA_d[:, blk, :], vast.t[:], [vast.b], [db("VA", g)])
                for j in range(24 if PD & 4 else 0):
                    pa = PS.next()
                    W_ = Wfm[29 + j]
                    for kc in range(8):
                        MM(pa.t[:], W_.t[:, kc, :], xT.t[:, kc, :], kc == 0, kc == 7, [W_.b, xT.b], [pa.b])
                    ost = ost_r.next()
                    ACT(ost.t[:], pa.t[:], AF.Sigmoid, [pa.b], [ost.b])
                    DMA("sp", GT_d[:, j, ts], ost.t[:], [ost.b], [db("GT", g)])
                for tb in range(4 if PD & 8 else 0):
                    blk = g * 4 + tb
                    pv = PS.next()
                    for kc in range(8):
                        MM(pv.t[:, 0:NTM], xT.t[:, kc, tb * 128:(tb + 1) * 128], Wtm.t[:, kc, :], kc == 0, kc == 7, [xT.b, Wtm.b], [pv.b])
                    vbst = vbst_r.next()
                    vcst = vcst_r.next()
                    iwst = iwst_r.next()
                    TMO = int(os.environ.get("TMO", "7"))
                    if TMO & 1:
                        CP("act", vbst.t[:, :, 0:64], pv.t[:, 0:256].rearrange("p (h d) -> p h d", h=4), [pv.b], [vbst.b])
                    if TMO & 2:
                        CP("dve", vcst.t[:, :, 0:64], pv.t[:, 256:384].rearrange("p (h d) -> p h d", h=2), [pv.b], [vcst.b])
                    if TMO & 4:
                        CP("dve", iwst.t[:, 4:8], pv.t[:, 384:388], [pv.b], [iwst.b])
                        STT(iwst.t[:, 0:4], pv.t[:, 384:388], -1.0, iwst.t[:, 4:8], ALU.mult, ALU.max, [pv.b, iwst.b], [iwst.b])
                    TMD = int(os.environ.get("TMD", "15"))
                    if TMD & 1:
                        ACT(iwst.t[:, 4:8], pv.t[:, 384:388], AF.Sign, [pv.b], [iwst.b])
                    if TMD & 2:
                        DMA("sp", VB_d[:, blk, :], vbst.t[:].rearrange("p h d -> p (h d)"), [vbst.b], [db("VB", g)])
                    if TMD & 4:
                        DMA("sp", VC_d[blk * 128:(blk + 1) * 128, :], vcst.t[:].rearrange("p h d -> p (h d)"), [vcst.b], [db("VC", blk)])
                    if TMD & 8:
                        DMA("sp", IW_d[blk * 128:(blk + 1) * 128, :], iwst.t[:], [iwst.b], [db("IW", blk)])
            S.barrier()

        if "A1" in stages:
            A.reset(base_mark)
            KI = A.tile("KI", [128, S_LEN], BF16)
            ka_b, ik_b = Buf("ka"), Buf("ik")
            VA = A.tile("VA", [128, NB, 65], BF16)
            KBt = A.tile("KB", [128, 2, S_LEN], BF16)
            VBt = A.tile("VB", [128, NB, 260], BF16)
            acc = A.tile("acc", [128, S_LEN], F32)
            mb = A.tile("mb", [128, S_LEN], BF16)
            kms = A.tile("kms", [128, 2, 32], F32)
            kmT = A.tile("kmT", [128, 2, 32], BF16)
            gs = A.tile("gs", [128, 4, 32], F32)
            m8 = A.tile("m8", [128, 4, 8], F32)
            mbm = A.tile("mbm", [128, 4, 32], BF16)
            mbT = A.tile("mbT", [32, 512], BF16)
            QI_r = A.ring("QI", [128, 4, 128], BF16, 2)
            QA_r = A.ring("QAz", [128, 4, 128], BF16, 2)
            QB_r = A.ring("QB", [128, 4, 128], BF16, 2)
            for rg in (QI_r, QA_r, QB_r):
                for tl in rg.items:
                    MSET("dve", tl.t[:], 0.0, [tl.b])
            IW_r = A.ring("IW", [128, 8], F32, 2)
            pT_r = A.ring("pT", [128, 512], BF16, 4)
            oab_r = A.ring("oab", [128, 512], BF16, 2)
            bs = A.tile("bs", [128, 8], F32)
            HW = A.tile("HW", [128, NBIS], F32)
            rec = A.tile("rec", [128, 8], F32)

            DMA("sp", KI.t[0:64, :], KA_d, dbg_all("KA"), [ka_b])
            DMA("sp", KI.t[64:128, :], IK_d, dbg_all("IK"), [ik_b])
            DMA("sp", VA.t[:], VA_d, dbg_all("VA"), [VA.b])
            for c in range(2):
                DMA("sp", KBt.t[:, c, :], KB_d[:, c, :], dbg_all("KB"), [KBt.b])
            for c in range(4):
                DMA("sp", VBt.t[:, c * 16:(c + 1) * 16, :], VB_d[:, c * 16:(c + 1) * 16, :], dbg_all("VB"), [VBt.b])
            MSET("dve", gs.t[:], -BIG, [gs.b])
            for c in range(2):
                RED(kms.t[:, c, :], KBt.t[:, c, :].rearrange("p (n k) -> p n k", k=256), ALU.add, [KBt.b], [kms.b])
            TS("dve", kmT.t[:], kms.t[:], 1.0 / 256.0, ALU.mult, [kms.b], [kmT.b])

            def load_a1(i):
                QI = QI_r.next()
                QAz = QA_r.next()
                QB = QB_r.next()
                IW = IW_r.next()
                bsl = slice(i * 128, (i + 1) * 128)
                g = i // 4
                DMA("sp", QAz.t[0:64, :, :], QA_d[:, :, bsl], [db("QA", g)], [QAz.b])
                DMA("sp", QI.t[64:128, :, :], IQ_d[:, :, bsl], [db("IQ", g)], [QI.b])
                for h in range(4):
                    p0 = (h % 2) * 64
                    DMA("sp", QB.t[p0:p0 + 64, h, :], QB_d[p0:p0 + 64, h // 2, bsl], [db("QB", g)], [QB.b])
                DMA("sp", IW.t[:], IW_d[bsl, :], [db("IW", i)], [IW.b])
                return QI, QAz, QB, IW

            import os
            NB1 = int(os.environ.get("A1NB", str(NB)))
            AP_ = int(os.environ.get("A1PARTS", "31"))
            nxt = load_a1(0)
            for i in range(NB1):
                QI, QAz, QB, IW = nxt
                if i + 1 < NB1:
                    nxt = load_a1(i + 1)
                nk = 128 * (i + 1)
                cur = i // 2
                if cur > 0 and (AP_ & 1):
                    pg = PS.next()
                    for h in range(4):
                        p0 = (h % 2) * 64
                        MM(pg.t[:, h * 32:(h + 1) * 32], QB.t[:, h, :], kmT.t[:, h // 2, :],
                           h == 0, h == 3, [QB.b, kmT.b], [pg.b], skip=True)
                    CP("dve", gs.t[:, :, 0:cur], pg.t[:, 0:128].rearrange("p (h n) -> p h n", h=4)[:, :, 0:cur], [pg.b], [gs.b])
                    for h in range(4):
                        MAX8(m8.t[:, h, :], gs.t[:, h, :], [gs.b], [m8.b])
                    for h in range(4):
                        TS("dve", mbm.t[:, h, :], gs.t[:, h, :], m8.t[:, h, 2:3], ALU.is_lt, [gs.b, m8.b], [mbm.b], s2=NEG, op1=ALU.mult)
                    pt = PS.next()
                    ptv = pt.t[:].bitcast(BF16)
                    for h in range(4):
                        TR(ptv[0:32, h * 128:(h + 1) * 128], mbm.t[:, h, :], ident_bf.t[:], [mbm.b, ident_bf.b], [pt.b])
                    CP("act", mbT.t[:], ptv[0:32, 0:512], [pt.b], [mbT.b])
                if i >= 2 and (AP_ & 2):
                    ntile = (nk + 511) // 512
                    for j in range(ntile):
                        w = min(512, nk - 512 * j)
                        ks = slice(j * 512, j * 512 + w)
                        for h in range(4):
                            pi = PS.next()
                            MM(pi.t[:, 0:w], QI.t[:, h, :], KI.t[:, ks], True, True, [QI.b, ik_b, ka_b], [pi.b])
                            ACT(pi.t[:, 0:w], pi.t[:, 0:w], AF.Relu, [pi.b, IW.b], [pi.b], scale=IW.t[:, h:h + 1])
                            if h == 0:
                                TS("dve", acc.t[:, ks], pi.t[:, 0:w], IW.t[:, 4:5], ALU.mult, [pi.b, IW.b], [acc.b])
                            else:
                                STT(acc.t[:, ks], pi.t[:, 0:w], IW.t[:, 4 + h:5 + h], acc.t[:, ks], ALU.mult, ALU.add, [pi.b, IW.b, acc.b], [acc.b])
                    dsl = slice(i * 128, (i + 1) * 128)
                    TT("dve", acc.t[:, dsl], acc.t[:, dsl], trineg.t[:], ALU.add, [acc.b, trineg.b], [acc.b])
                    RED(bs.t[:, 5:6], acc.t[:, 0:nk], ALU.max, [acc.b], [bs.b])
                    RED(bs.t[:, 0:1], acc.t[:, 0:i * 128], ALU.min, [acc.b], [bs.b])
                    TT("dve", bs.t[:, 1:2], bs.t[:, 5:6], bs.t[:, 0:1], ALU.subtract, [bs.b], [bs.b])
                    TS("dve", HW.t[:], cpow.t[:], bs.t[:, 1:2], ALU.mult, [cpow.b, bs.b], [HW.b])
                    TT("dve", bs.t[:, 2:3], bs.t[:, 0:1], HW.t[:, 0:1], ALU.add, [bs.b, HW.b], [bs.b])
                    for k in range(NBIS):
                        TS("dve", mb.t[:, 0:nk], acc.t[:, 0:nk], bs.t[:, 2:3], ALU.is_ge, [acc.b, bs.b], [mb.b, bs.b],
                           s2=None, op1=ALU.add, accum=bs.t[:, 3:4])
                        if k < NBIS - 1:
                            TS("dve", bs.t[:, 4:5], bs.t[:, 3:4], TOPK - 0.5, ALU.is_ge, [bs.b], [bs.b], s2=0.5, op1=ALU.subtract)
                            STT(bs.t[:, 2:3], bs.t[:, 4:5], HW.t[:, k:k + 1], bs.t[:, 2:3], ALU.mult, ALU.add, [bs.b, HW.b], [bs.b])
                        else:
                            TS("dve", bs.t[:, 4:5], bs.t[:, 3:4], TOPK - 0.5, ALU.is_ge, [bs.b], [bs.b], s2=1.0, op1=ALU.subtract)
                            STT(bs.t[:, 0:1], bs.t[:, 4:5], HW.t[:, k:k + 1], bs.t[:, 2:3], ALU.mult, ALU.add, [bs.b, HW.b], [bs.b])
                    thr = bs.t[:, 0:1]
                    thr_b = bs.b
                else:
                    MSET("dve", acc.t[:, 0:nk], 0.0, [acc.b])
                    dsl = slice(i * 128, (i + 1) * 128)
                    TT("dve", acc.t[:, dsl], acc.t[:, dsl], trineg.t[:], ALU.add, [acc.b, trineg.b], [acc.b])
                    thr = neg1.t[:, 0:1]
                    thr_b = neg1.b
                TS("dve", mb.t[:, 0:nk], acc.t[:, 0:nk], thr, ALU.is_lt, [acc.b, thr_b], [mb.b], s2=NEG, op1=ALU.mult)
                OB = PO.next()
                for kb in range(i + 1 if AP_ & 4 else 0):
                    ks = slice(kb * 128, (kb + 1) * 128)
                    pst = PS.next()
                    first = True
                    if kb < 2 * cur:
                        n = kb // 2
                        MM(pst.t[:], Ec.t[:, n * 128:(n + 1) * 128], mbT.t[:], True, False, [Ec.b, mbT.b], [pst.b], skip=True)
                        first = False
                    elif kb == i:
                        MM(pst.t[:], ident_bf.t[:], tri4.t[:], True, False, [ident_bf.b, tri4.b], [pst.b], skip=True)
                        first = False
                    for h in range(4):
                        p0 = (h % 2) * 64
                        MM(pst.t[:, h * 128:(h + 1) * 128], KBt.t[:, h // 2, ks], QB.t[:, h, :],
                           first and h == 0, h == 3, [KBt.b, QB.b], [pst.b], skip=True)
                    pT = pT_r.next()
                    ACT(pT.t[:], pst.t[:], AF.Exp, [pst.b], [pT.b], scale=0.125)
                    for h in range(4):
                        MM(OB.t[:, h * 65:(h + 1) * 65], pT.t[:, h * 128:(h + 1) * 128], VBt.t[:, kb, h * 65:(h + 1) * 65],
                           kb == 0 and h == 0, kb == i and h == 3, [pT.b, VBt.b], [OB.b], skip=True)
                OA = PO.next()
                for kb in range(i + 1 if AP_ & 8 else 0):
                    ks = slice(kb * 128, (kb + 1) * 128)
                    pst = PS.next()
                    MM(pst.t[:], mb.t[:, ks], i4.t[:], True, False, [mb.b, i4.b], [pst.b])
                    MM(pst.t[:], KI.t[:, ks], QAz.t[:].rearrange("p h q -> p (h q)"), False, True, [ka_b, ik_b, QAz.b], [pst.b])
                    pT = pT_r.next()
                    ACT(pT.t[:], pst.t[:], AF.Exp, [pst.b], [pT.b], scale=0.125)
                    for h in range(4):
                        MM(OA.t[:, h * 65:(h + 1) * 65], pT.t[:, h * 128:(h + 1) * 128], VA.t[:, kb, :],
                           kb == 0 and h == 0, kb == i and h == 3, [pT.b, VA.b], [OA.b], skip=True)
                oab = oab_r.next()
                for (O_, c0, r0) in (((OA, 0, 0), (OB, 256, 4)) if AP_ & 16 else ()):
                    ov = O_.t[:, 0:260].rearrange("p (h d) -> p h d", d=65)
                    RECIP(rec.t[:, r0:r0 + 4], ov[:, :, 64], [O_.b], [rec.b])
                    for h in range(4):
                        TS("dve", oab.t[:, c0 + h * 64:c0 + (h + 1) * 64], ov[:, h, 0:64], rec.t[:, r0 + h:r0 + h + 1], ALU.mult,
                           [O_.b, rec.b], [oab.b])
                DMA("sp", OAB_d[i * 128:(i + 1) * 128, :], oab.t[:], [oab.b], [db("OAB", i)])
            S.barrier()

        if "A2" in stages:
            A.reset(base_mark)
            wbr = A.tile("wbr", [128, 8, D], BF16)
            wo = A.tile("wo", [128, 8, D], BF16)
            g1 = A.tile("g1", [128, D], F32)
            b1 = A.tile("b1", [128, D], F32)
            esink = A.tile("esink", [128, 8], F32)
            QC_r = A.ring("QC", [128, 2, 4, 128], BF16, 2)
            import os
            for tl in (QC_r.items if not int(os.environ.get("A2SKIP", "0")) & 8 else []):
                MSET("dve", tl.t[:], 0.0, [tl.b])
            KC_r = [A.tile(f"KC{k}", [128, 128], BF16) for k in range(3)]
            VC_r = [A.tile(f"VC{k}", [128, 130], BF16) for k in range(3)]
            gT_r = A.ring("gT", [128, 24, 128], BF16, 2)
            xb_r2 = A.ring("xblk", [128, D], F32, 2)
            oall_r = A.ring("oall", [128, D], BF16, 2)
            oT_r = A.ring("oT", [128, 8, 128], BF16, 2)
            tmp_r = A.ring("mtmp", [128, 3, 128], F32, 2)
            mT_r = A.ring("mT", [128, 8, 128], BF16, 2)
            r_r = A.ring("r", [128, D], F32, 2)
            xo_r = A.ring("xo", [128, D], F32, 2)
            pT_r = A.ring("pT2", [128, 512], BF16, 3)
            rec = A.tile("rec2", [128, 8], F32)
            lnt = {"st": A.tile("st", [128, 2, 6], F32), "mv": A.tile("mv", [128, 2], F32), "sd": A.tile("sd", [128, 4], F32)}
            import os
            SK = int(os.environ.get("A2SKIP", "0"))
            if not SK & 1:
                DMA("pool", wbr.t[:], wbr_d[l].rearrange("(kc p) n -> p kc n", p=128), [], [wbr.b])
                DMA("pool", wo.t[:], wo_d[l].rearrange("(kc p) n -> p kc n", p=128), [], [wo.b])
            if not SK & 2:
                DMA("sp", g1.t[:], lnp_d[l, 0], [], [g1.b])
                DMA("sp", b1.t[:], lnp_d[l, 1], [], [b1.b])
                DMA("sp", esink.t[:], sinks_d[l], [], [esink.b])
            if not SK & 4:
                ACT(esink.t[:], esink.t[:], AF.Exp, [esink.b], [esink.b])

            def load_a2(i):
                bsl = slice(i * 128, (i + 1) * 128)
                g = i // 4
                QC = QC_r.next()
                gT = gT_r.next()
                xblk = xb_r2.next()
                oall = oall_r.next()
                KC = KC_r[i % 3]
                VC = VC_r[i % 3]
                oc_b = Buf("oall_c")
                for gq in range(2):
                    DMA("sp", QC.t[gq * 64:(gq + 1) * 64, gq, :, :], QC_d[gq * 64:(gq + 1) * 64, :, bsl], [db("QC", g)], [QC.b])
                DMA("sp", KC.t[:], KC_d[:, bsl], [db("KC", g)], [KC.b])
                DMA("sp", VC.t[:], VC_d[bsl, :], [db("VC", i)], [VC.b])
                DMA("sp", gT.t[:], GT_d[:, :, bsl], [db("GT", g)], [gT.b])
                DMA("sp", xblk.t[:], xin_d[bsl, :], [db(xin_name, i)], [xblk.b])
                DMA("sp", oall.t[:, 0:512], OAB_d[bsl, :], [db("OAB", i), oc_b], [oall.b])
                return QC, gT, xblk, oall, oc_b

            import os
            NB2 = int(os.environ.get("A2NB", str(NB)))
            nxt = load_a2(0) if NB2 > 0 else None
            for i in range(NB2):
                QC, gT, xblk, oall, oc_b = nxt
                if i + 1 < NB2:
                    nxt = load_a2(i + 1)
                for gq in range(2):
                    OC = PO.next()
                    kbs = ([i - 1] if i > 0 else []) + [i]
                    for kb in kbs:
                        KC = KC_r[kb % 3]
                        VC = VC_r[kb % 3]
                        pst = PS.next()
                        mk = band4 if kb == i - 1 else tri4
                        MM(pst.t[:], ident_bf.t[:], mk.t[:], True, False, [ident_bf.b, mk.b], [pst.b])
                        MM(pst.t[:], KC.t[:, :], QC.t[:, gq, :, :].rearrange("p h q -> p (h q)"),
                           False, True, [KC.b, QC.b], [pst.b])
                        pT = pT_r.next()
                        ACT(pT.t[:], pst.t[:], AF.Exp, [pst.b], [pT.b], scale=0.125)
                        for r in range(4):
                            MM(OC.t[:, r * 65:(r + 1) * 65], pT.t[:, r * 128:(r + 1) * 128], VC.t[:, gq * 65:(gq + 1) * 65],
                               kb == kbs[0] and r == 0, kb == i and r == 3, [pT.b, VC.b], [OC.b], skip=True)
                    ov = OC.t[:, 0:260].rearrange("p (h d) -> p h d", d=65)
                    TT("dve", rec.t[:, 0:4], ov[:, :, 64], esink.t[:, gq * 4:(gq + 1) * 4], ALU.add, [OC.b, esink.b], [rec.b])
                    RECIP(rec.t[:, 4:8], rec.t[:, 0:4], [rec.b], [rec.b])
                    for r in range(4):
                        c0 = 512 + (gq * 4 + r) * 64
                        TS("dve", oall.t[:, c0:c0 + 64], ov[:, r, 0:64], rec.t[:, 4 + r:5 + r], ALU.mult, [OC.b, rec.b], [oc_b])
                pt = PS.next()
                ptv = pt.t[:].bitcast(BF16).rearrange("p (k q) -> p k q", k=8)
                for kc in range(8):
                    TR(ptv[:, kc, :], oall.t[:, kc * 128:(kc + 1) * 128], ident_bf.t[:], [oall.b, oc_b, ident_bf.b], [pt.b])
                oT = oT_r.next()
                CP("act", oT.t[:], ptv, [pt.b], [oT.b])
                if debug and l == 0:
                    DMA("sp", DBGo_d[i * 128:(i + 1) * 128, :], oall.t[:], [oall.b, oc_b], [db("DBGo", i)])
                mT = mT_r.next()
                for m in range(8):
                    pm = PS.next()
                    ms = slice(m * 128, (m + 1) * 128)
                    for (br, kcs) in ((0, (0, 1)), (1, (2, 3)), (2, (4, 5, 6, 7))):
                        for kc in kcs:
                            MM(pm.t[:, br * 128:(br + 1) * 128], wbr.t[:, kc, ms], oT.t[:, kc, :], kc == 0, kc == 7,
                               [wbr.b, oT.b], [pm.b], skip=True)
                    tmp = tmp_r.next()
                    TT("dve", tmp.t[:], pm.t[:, 0:384].rearrange("p (b q) -> p b q", b=3), gT.t[:, m:24:8, :], ALU.mult, [pm.b, gT.b], [tmp.b])
                    RED(mT.t[:, m, :], tmp.t[:].rearrange("p b q -> p q b"), ALU.add, [tmp.b], [mT.b])
                r_ = r_r.next()
                for nh in range(2):
                    py = PS.next()
                    ns = slice(nh * 512, (nh + 1) * 512)
                    for m in range(8):
                        MM(py.t[:], mT.t[:, m, :], wo.t[:, m, ns], m == 0, m == 7, [mT.b, wo.b], [py.b])
                    STT(r_.t[:, ns], xblk.t[:, ns], DN_ALPHA, py.t[:], ALU.mult, ALU.add, [xblk.b, py.b], [r_.b])
                if debug and l == 0:
                    DMA("sp", DBGm_d[:, :, i * 128:(i + 1) * 128], mT.t[:], [mT.b], [db("DBGm", i)])
                    DMA("sp", DBGr_d[i * 128:(i + 1) * 128, :], r_.t[:], [r_.b], [db("DBGr", i)])
                xo = xo_r.next()
                layer_norm(r_, g1, b1, xo, lnt)
                DMA("sp", X1_d[i * 128:(i + 1) * 128, :], xo.t[:], [xo.b], [db("X1", i)])
            S.barrier()

        if "M" in stages:
            A.reset(base_mark)
            NQB = 16
            accs = [A.tile(f"macc{k}", [128, D], F32) for k in range(NQB)]
            x1T = A.tile("x1T", [128, 8, NQB * 128], BF16)
            W_r = A.ring("mw", [128, 4096], BF16, 5)
            hT_r = A.ring("hT", [128, 4, 512], BF16, 2)
            sg_r = A.ring("sg", [128, 512], BF16, 2)
            x1f_r = A.ring("x1f", [128, D], F32, 2)
            x1Tf_r = A.ring("x1Tf", [128, 8, 128], F32, 2)
            cw = A.tile("cw", [128, NQB, NE], F32)
            g2 = A.tile("g2", [128, D], F32)
            b2 = A.tile("b2", [128, D], F32)
            xo_r = A.ring("xo2", [128, D], F32, 2)
            lnt = {"st": A.tile("st2", [128, 2, 6], F32), "mv": A.tile("mv2", [128, 2], F32), "sd": A.tile("sd2", [128, 4], F32)}
            rt = {k: A.tile(f"rt_{k}", [128, NE], F32) for k in ("aff", "sel", "selm", "sel2", "e1", "e2")}
            rs = A.tile("rsm", [128, 16], F32)
            DMA("sp", g2.t[:], lnp_d[l, 2], [], [g2.b])
            DMA("sp", b2.t[:], lnp_d[l, 3], [], [b2.b])
            for qt in range(S_LEN // (NQB * 128)):
                for k in range(NQB):
                    blk = qt * NQB + k
                    x1f = x1f_r.next()
                    DMA("sp", x1f.t[:], X1_d[blk * 128:(blk + 1) * 128, :], [db("X1", blk)], [x1f.b])
                    x1Tf = x1Tf_r.next()
                    for half in range(2):
                        pt = PS.next()
                        for c in range(4):
                            kc = half * 4 + c
                            TR(pt.t[:, c * 128:(c + 1) * 128], x1f.t[:, kc * 128:(kc + 1) * 128], ident_f.t[:], [x1f.b, ident_f.b], [pt.b])
                        CP("act", x1Tf.t[:, half * 4:(half + 1) * 4, :], pt.t[:].rearrange("p (c q) -> p c q", c=4), [pt.b], [x1Tf.b])
                        CP("dve", x1T.t[:, half * 4:(half + 1) * 4, k * 128:(k + 1) * 128], pt.t[:].rearrange("p (c q) -> p c q", c=4), [pt.b], [x1T.b])
                    ACT(accs[k].t[:], x1f.t[:], AF.Copy, [x1f.b], [accs[k].b], scale=DN_ALPHA)
                    pr = PS.next()
                    for kc in range(8):
                        MM(pr.t[:, 0:NE], x1Tf.t[:, kc, :], rw.t[:, kc, :], kc == 0, kc == 7, [x1Tf.b, rw.b], [pr.b])
                    aff, sel, selm, sel2, e1, e2 = (rt[n] for n in ("aff", "sel", "selm", "sel2", "e1", "e2"))
                    ACT(aff.t[:], pr.t[:, 0:NE], AF.Sigmoid, [pr.b], [aff.b])
                    TT("dve", sel.t[:], aff.t[:], rb.t[:], ALU.add, [aff.b, rb.b], [sel.b])
                    sel3 = sel.t[:].rearrange("p (g e) -> p g e", g=4)
                    RED(rs.t[:, 0:4], sel3, ALU.max, [sel.b], [rs.b])
                    for gq in range(4):
                        TS("dve", e1.t[:, gq * 4:(gq + 1) * 4], sel.t[:, gq * 4:(gq + 1) * 4], rs.t[:, gq:gq + 1], ALU.is_equal,
                           [sel.b, rs.b], [e1.b])
                    STT(sel2.t[:], e1.t[:], -BIG, sel.t[:], ALU.mult, ALU.add, [e1.b, sel.b], [sel2.b])
                    RED(rs.t[:, 4:8], sel2.t[:].rearrange("p (g e) -> p g e", g=4), ALU.max, [sel2.b], [rs.b])
                    TT("dve", rs.t[:, 8:12], rs.t[:, 0:4], rs.t[:, 4:8], ALU.add, [rs.b], [rs.b])
                    RED(rs.t[:, 12:13], rs.t[:, 8:12], ALU.max, [rs.b], [rs.b])
                    TS("dve", rs.t[:, 8:12], rs.t[:, 8:12], rs.t[:, 12:13], ALU.is_equal, [rs.b], [rs.b])
                    TS("dve", rs.t[:, 8:12], rs.t[:, 8:12], -1.0, ALU.add, [rs.b], [rs.b], s2=BIG, op1=ALU.mult)
                    for gq in range(4):
                        TS("dve", selm.t[:, gq * 4:(gq + 1) * 4], sel.t[:, gq * 4:(gq + 1) * 4], rs.t[:, 8 + gq:9 + gq], ALU.add,
                           [sel.b, rs.b], [selm.b])
                    RED(rs.t[:, 13:14], selm.t[:], ALU.max, [selm.b], [rs.b])
                    TS("dve", e1.t[:], selm.t[:], rs.t[:, 13:14], ALU.is_equal, [selm.b, rs.b], [e1.b])
                    STT(sel2.t[:], e1.t[:], -BIG, selm.t[:], ALU.mult, ALU.add, [e1.b, selm.b], [sel2.b])
                    RED(rs.t[:, 14:15], sel2.t[:], ALU.max, [sel2.b], [rs.b])
                    TS("dve", e2.t[:], sel2.t[:], rs.t[:, 14:15], ALU.is_equal, [sel2.b, rs.b], [e2.b])
                    TT("dve", e1.t[:], e1.t[:], e2.t[:], ALU.add, [e1.b, e2.b], [e1.b])
                    TT("dve", e1.t[:], e1.t[:], aff.t[:], ALU.mult, [e1.b, aff.b], [e1.b])
                    RED(rs.t[:, 15:16], e1.t[:], ALU.add, [e1.b], [rs.b])
                    RECIP(rs.t[:, 15:16], rs.t[:, 15:16], [rs.b], [rs.b])
                    TS("dve", cw.t[:, k, :], e1.t[:], rs.t[:, 15:16], ALU.mult, [e1.b, rs.b], [cw.b])
                for e in range(NE):
                    wg = W_r.next()
                    wu = W_r.next()
                    wd = W_r.next()
                    DMA("pool", wg.t[:].rearrange("p (k n) -> p k n", k=8), wg_d[l, e].rearrange("(kc p) n -> p kc n", p=128), [], [wg.b])
                    DMA("pool", wu.t[:].rearrange("p (k n) -> p k n", k=8), wu_d[l, e].rearrange("(kc p) n -> p kc n", p=128), [], [wu.b])
                    DMA("pool", wd.t[:].rearrange("p (k n) -> p k n", k=4), wd_d[l, e].rearrange("(kc p) n -> p kc n", p=128), [], [wd.b])
                    wgv = wg.t[:].rearrange("p (k n) -> p k n", k=8)
                    wuv = wu.t[:].rearrange("p (k n) -> p k n", k=8)
                    wdv = wd.t[:].rearrange("p (k n) -> p k n", k=4)
                    for tg in range(NQB // 4):
                        tsl = slice(tg * 512, (tg + 1) * 512)
                        hT = hT_r.next()
                        for mc in range(4):
                            msl = slice(mc * 128, (mc + 1) * 128)
                            pg = PS.next()
                            pu = PS.next()
                            for kc in range(8):
                                MM(pg.t[:], wgv[:, kc, msl], x1T.t[:, kc, tsl], kc == 0, kc == 7, [wg.b, x1T.b], [pg.b])
                            for kc in range(8):
                                MM(pu.t[:], wuv[:, kc, msl], x1T.t[:, kc, tsl], kc == 0, kc == 7, [wu.b, x1T.b], [pu.b])
                            sg = sg_r.next()
                            ACT(sg.t[:], pg.t[:], AF.Silu, [pg.b], [sg.b])
                            TT("dve", hT.t[:, mc, :], sg.t[:], pu.t[:], ALU.mult, [sg.b, pu.b], [hT.b])
                        for tb in range(4):
                            k = tg * 4 + tb
                            for nh in range(2):
                                ns = slice(nh * 512, (nh + 1) * 512)
                                py = PS.next()
                                for mc in range(4):
                                    MM(py.t[:], hT.t[:, mc, tb * 128:(tb + 1) * 128], wdv[:, mc, ns], mc == 0, mc == 3, [hT.b, wd.b], [py.b])
                                STT(accs[k].t[:, ns], py.t[:], cw.t[:, k, e:e + 1], accs[k].t[:, ns], ALU.mult, ALU.add,
                                    [py.b, cw.b, accs[k].b], [accs[k].b])
                for k in range(NQB):
                    blk = qt * NQB + k
                    xo = xo_r.next()
                    layer_norm(accs[k], g2, b2, xo, lnt)
                    if l == depth - 1:
                        out_ops.append(DMA("sp", out_d[blk * 128:(blk + 1) * 128, :], xo.t[:], [xo.b], [db("out", blk)]))
                    else:
                        DMA("sp", XL_d[blk * 128:(blk + 1) * 128, :], xo.t[:], [xo.b], [db("XL", blk)])
            S.barrier()

    if not out_ops:
        out_ops = list(S.dma_last.values())
    S.emit(out_ops)
    global _LAST_S
    _LAST_S = S
    return nc


def _bf(a):
    return np.asarray(a, dtype=np.float32).astype(ml_dtypes.bfloat16)


def _consts():
    c = {}
    eye = np.eye(128, dtype=np.float32)
    c["c_ident_bf"] = _bf(eye)
    c["c_ident_f"] = eye
    c["c_i4"] = _bf(np.tile(eye, (1, 4)))
    key = np.arange(128)[:, None]
    q = np.arange(128)[None, :]
    tri = np.where(key > q, NEG, 0.0).astype(np.float32)
    band = np.where(key <= q, NEG, 0.0).astype(np.float32)
    c["c_tri4"] = _bf(np.tile(tri, (1, 4)))
    c["c_band4"] = _bf(np.tile(band, (1, 4)))
    c["c_trineg"] = np.where(q > key, -BIG, 0.0).astype(np.float32)
    E = np.zeros((32, 4096), np.float32)
    for n in range(32):
        E[n, n * 128:(n + 1) * 128] = 1.0
    c["c_E"] = _bf(E)
    c["c_pow"] = np.tile((0.5 ** np.arange(1, NBIS + 1, dtype=np.float64)).astype(np.float32)[None, :], (128, 1))
    inv = 1.0 / (10000.0 ** (np.arange(0, 64, 2, dtype=np.float32) / np.float32(64)))
    ang = np.arange(S_LEN, dtype=np.float32)[:, None] * inv[None, :].astype(np.float32)
    cos = np.cos(ang).astype(np.float32)
    sin = np.sin(ang).astype(np.float32)
    p = np.arange(128)
    cosT = cos[:, p % 32].T
    sgn = np.where((p % 64) < 32, -1.0, 1.0).astype(np.float32)
    sinT = (sin[:, p % 32].T) * sgn[:, None]
    c["cosT"] = np.ascontiguousarray(cosT, dtype=np.float32)
    c["sinT"] = np.ascontiguousarray(sinT, dtype=np.float32)
    return c


def _col_plan():
    offs = np.concatenate([[0], np.cumsum(IN_SIZES)])
    o_aq, o_ac, o_iq, o_ik, o_iw, o_bq, o_bk, o_bv, o_cq, o_ck, o_cv, o_g = offs[:12]

    def heads(c0, h0, nh):
        main, swp = [], []
        for h in range(h0, h0 + nh):
            for d in range(64):
                main.append(c0 + h * 64 + d)
                swp.append(c0 + h * 64 + (d + 32) % 64)
        return main, swp

    units = []
    units += [heads(o_aq, 0, 2), heads(o_aq, 2, 2)]
    units += [heads(o_iq, 0, 2), heads(o_iq, 2, 2)]
    m, s = heads(o_ik, 0, 1)
    units += [(m + m, s + s)]
    units += [heads(o_bq, 0, 2), heads(o_bq, 2, 2)]
    units += [heads(o_bk, 0, 2), heads(o_bk, 2, 2)]
    units += [heads(o_cq, 2 * c, 2) for c in range(4)]
    units += [heads(o_ck, 0, 2)]
    chunks = []
    for (m, s) in units:
        chunks.append(np.array(m))
        chunks.append(np.array(s))
    chunks.append(np.arange(o_ac, o_ac + 128))
    for j in range(24):
        chunks.append(np.arange(o_g + 128 * j, o_g + 128 * (j + 1)))
    assert len(chunks) == NFM
    tm = np.concatenate([np.arange(o_bv, o_bv + 256), np.arange(o_cv, o_cv + 128), np.arange(o_iw, o_iw + 4)])
    return chunks, tm


def _prep_shared(inp):
    f = lambda a: np.ascontiguousarray(np.asarray(a, dtype=np.float32))
    w_in = f(inp["w_in"])
    chunks, tm = _col_plan()
    w_fm = np.empty((DEPTH, NFM, 128, 8, 128), np.float32)
    w_tm = np.empty((DEPTH, 128, 8, NTM), np.float32)
    for l in range(DEPTH):
        wl = w_in[l].reshape(8, 128, N_IN)
        for ci, idx in enumerate(chunks):
            w_fm[l, ci] = wl[:, :, idx].transpose(1, 0, 2)
        w_tm[l] = wl[:, :, tm].transpose(1, 0, 2)
    sw = np.array([(d + 32) % 64 for d in range(64)])
    uk = f(inp["a_w_uk"])
    sh = {
        "w_fm": w_fm, "w_tm": w_tm, "uk": uk, "uksw": np.ascontiguousarray(uk[:, :, sw]), "uv": f(inp["a_w_uv"]),
        "w_branch": f(inp["w_branch"]), "w_o": f(inp["w_o"]),
        "moe_w_gate": f(inp["moe_w_gate"]), "moe_w_up": f(inp["moe_w_up"]), "moe_w_down": f(inp["moe_w_down"]),
    }
    lnp = np.empty((DEPTH, 4, 128, D), np.float32)
    for l in range(DEPTH):
        for k, nm in enumerate(("ln1_g", "ln1_b", "ln2_g", "ln2_b")):
            lnp[l, k] = np.broadcast_to(f(inp[nm])[l][None, :], (128, D))
    sh["lnp"] = lnp
    sh["sinks_bc"] = np.ascontiguousarray(np.broadcast_to(f(inp["c_sinks"])[:, None, :], (DEPTH, 128, 8)))
    sh["rw"] = np.ascontiguousarray(f(inp["router_w"]).reshape(8, 128, NE).transpose(1, 0, 2))
    sh["rb_bc"] = np.ascontiguousarray(np.broadcast_to(f(inp["router_b"])[None, :], (128, NE)))
    sh.update(_consts())
    return sh


_NC_CACHE = {}


def kernel(**inputs):
    x = np.ascontiguousarray(np.asarray(inputs["x"], dtype=np.float32))
    sh = _prep_shared(inputs)
    if "nc" not in _NC_CACHE:
        _NC_CACHE["nc"] = build_program()
    nc = _NC_CACHE["nc"]
    in_maps = []
    for b in range(NCORES):
        m = dict(sh)
        m["x"] = x[b]
        in_maps.append(m)
    res = run_bass_kernel_spmd(nc, in_maps, core_ids=list(range(NCORES)))
    out = np.stack([np.asarray(res.results[b]["out"], dtype=np.float32) for b in range(NCORES)], axis=0)
    return out
```
